# Optimizing a Trainium2 kernel written in Bass

```python
import math
import jax, jax.numpy as jnp
from jax import lax
import numpy as np

D_MODEL = 2048
BATCH = 4
SEQ = 4096
DEPTH = 1
DEC_BATCH = 32
DEC_SEQ = 64
PAST_LEN = 2048

CHUNK = 64
Q_BLOCK = 128
D_CONV = D_MODEL // 2
CONV_WIDTH = 3
SB_HEADS = 8
SB_HEAD_DIM = 128
D_ATT = SB_HEADS * SB_HEAD_DIM
N_EXPERTS = 32
TOP_K = 4
D_FF = D_MODEL
SWIGLU_LIMIT = 7.0
SWIGLU_ALPHA = 1.702
LN_EPS = 1e-5
ALPHA = (2.0 * DEPTH) ** 0.25
BETA = (8.0 * DEPTH) ** -0.25
IN_WIDTHS = (D_CONV, D_CONV, D_CONV, D_ATT, D_ATT, D_ATT, D_MODEL, D_MODEL)
IN_SPLITS = tuple(int(s) for s in np.cumsum(IN_WIDTHS)[:-1])
IN_WIDTH = int(sum(IN_WIDTHS))

kernel_name = "hybrid_stickbreak_shortconv_moe_stream_step"


def layer_norm(x, g, b):
    xf = x.astype(jnp.float32)
    mu = jnp.mean(xf, axis=-1, keepdims=True)
    var = jnp.mean(jnp.square(xf - mu), axis=-1, keepdims=True)
    return ((xf - mu) * lax.rsqrt(var + LN_EPS) * g + b).astype(x.dtype)


def causal_conv3(xpad, w):
    n = xpad.shape[1] - (CONV_WIDTH - 1)
    return w[0] * xpad[:, :n] + w[1] * xpad[:, 1:n + 1] + w[2] * xpad[:, 2:]


def stick_breaking(q, k, v, q_start):
    bn, n, h, hd = q.shape
    s_len = k.shape[1]
    blk = min(Q_BLOCK, n)
    nb = n // blk
    scale = 1.0 / math.sqrt(hd)
    k_pos = jnp.arange(s_len)
    kf = k.astype(jnp.float32)
    vf = v.astype(jnp.float32)
    q_blocks = q.reshape(bn, nb, blk, h, hd).swapaxes(0, 1)
    q_pos = (q_start + jnp.arange(n)).reshape(nb, blk)

    def one_block(args):
        q_blk, qp = args
        z = jnp.einsum('bqhd,bshd->bhqs', q_blk.astype(jnp.float32), kf) * scale
        causal = k_pos[None, :] < qp[:, None]
        log_beta = jax.nn.log_sigmoid(z)
        log_keep = jnp.where(causal, jax.nn.log_sigmoid(-z), 0.0)
        rest = lax.cumsum(log_keep, axis=3, reverse=True) - log_keep
        w = jnp.where(causal, jnp.exp(log_beta + rest), 0.0)
        return jnp.einsum('bhqs,bshd->bqhd', w, vf)

    o = lax.map(one_block, (q_blocks, q_pos))
    return o.swapaxes(0, 1).reshape(bn, n, h, hd).astype(q.dtype)


def moe(h, w_router, b_router, w_gu, b_gu, w_dn, b_dn):
    logits = (h @ w_router + b_router).astype(jnp.float32)
    top_vals, top_idx = lax.top_k(logits, TOP_K)
    probs = jax.nn.softmax(top_vals, axis=-1)
    combine = jnp.sum(jax.nn.one_hot(top_idx, N_EXPERTS, dtype=jnp.float32) * probs[..., None], axis=1)

    def expert(acc, xs):
        w1, b1, w2, b2, g = xs
        gu = h @ w1 + b1
        gate = jnp.minimum(gu[:, :D_FF], SWIGLU_LIMIT)
        up = jnp.clip(gu[:, D_FF:], -SWIGLU_LIMIT, SWIGLU_LIMIT)
        out = ((up + 1.0) * gate * jax.nn.sigmoid(SWIGLU_ALPHA * gate)) @ w2 + b2
        return acc + g[:, None] * out.astype(jnp.float32), None

    acc0 = jnp.zeros(h.shape, jnp.float32)
    acc, _ = lax.scan(expert, acc0, (w_gu, b_gu, w_dn, b_dn, combine.T))
    return acc.astype(h.dtype)


def trunk_layer(u, c, conv_prev, k_past, v_past,
                w_ada, b_ada, w_in, conv_w, w_br_conv, w_br_att, w_out, ln1_g, ln1_b,
                w_router, b_router, w_gu, b_gu, w_dn, b_dn, ln2_g, ln2_b):
    bn, n, _ = u.shape
    past = k_past.shape[1]
    mod = (c @ w_ada + b_ada)[:, None, :]
    sh_t, sc_t, g_t, sh_f, sc_f, g_f = jnp.split(mod, 6, axis=-1)
    h = u * (1.0 + sc_t) + sh_t
    z = h @ w_in
    xc, bc, cc, q, k, v, ga, gb = jnp.split(z, IN_SPLITS, axis=-1)
    xin = cc * xc
    xpad = jnp.concatenate([conv_prev.astype(xin.dtype), xin], axis=1)
    yc = bc * causal_conv3(xpad, conv_w)
    q = q.reshape(bn, n, SB_HEADS, SB_HEAD_DIM)
    k = k.reshape(bn, n, SB_HEADS, SB_HEAD_DIM)
    v = v.reshape(bn, n, SB_HEADS, SB_HEAD_DIM)
    k_all = jnp.concatenate([k_past.astype(k.dtype), k], axis=1)
    v_all = jnp.concatenate([v_past.astype(v.dtype), v], axis=1)
    o = stick_breaking(q, k_all, v_all, past).reshape(bn, n, D_ATT)
    pc = yc @ w_br_conv
    pa = o @ w_br_att
    mix = (jax.nn.sigmoid(ga) * pc + jax.nn.sigmoid(gb) * pa) @ w_out
    u1 = layer_norm(ALPHA * u + (1.0 + g_t) * mix, ln1_g, ln1_b)
    h2 = u1 * (1.0 + sc_f) + sh_f
    f = moe(h2.reshape(bn * n, D_MODEL), w_router, b_router, w_gu, b_gu, w_dn, b_dn).reshape(bn, n, D_MODEL)
    u2 = layer_norm(ALPHA * u1 + (1.0 + g_f) * f, ln2_g, ln2_b)
    return u2, k, v, xpad[:, -(CONV_WIDTH - 1):]


def setup_inputs(seed: int = 0) -> dict:
    key = jax.random.key(seed)
    ks = jax.random.split(key, 32)
    f32 = jnp.float32

    def nrm(k, shape, s):
        return jax.random.normal(k, shape, f32) * s

    L, D = DEPTH, D_MODEL
    col_scale = jnp.concatenate([jnp.full((w,), s, f32) for w, s in zip(
        IN_WIDTHS, (BETA, 1.0, 1.0, 1.0, 1.0, BETA, 1.0, 1.0))])
    return {
        "x_prompt": nrm(ks[0], (BATCH, SEQ, D), 1.0),
        "x_sample": nrm(ks[1], (DEC_BATCH, DEC_SEQ, D), 1.0),
        "cache_k": nrm(ks[2], (L, DEC_BATCH, PAST_LEN, SB_HEADS, SB_HEAD_DIM), 1.0),
        "cache_v": nrm(ks[3], (L, DEC_BATCH, PAST_LEN, SB_HEADS, SB_HEAD_DIM), BETA),
        "cache_conv": nrm(ks[4], (L, DEC_BATCH, CONV_WIDTH - 1, D_CONV), BETA),
        "c_prompt": nrm(ks[5], (BATCH, D), 1.0),
        "c_sample": nrm(ks[6], (DEC_BATCH, D), 1.0),
        "ln0_g": 1.0 + nrm(ks[7], (D,), 0.02),
        "ln0_b": nrm(ks[8], (D,), 0.02),
        "w_ada": nrm(ks[9], (L, D, 6 * D), 0.1 * D ** -0.5),
        "b_ada": nrm(ks[10], (L, 6 * D), 0.02),
        "w_in": nrm(ks[11], (L, D, IN_WIDTH), D ** -0.5) * col_scale,
        "conv_w": nrm(ks[12], (L, CONV_WIDTH, D_CONV), CONV_WIDTH ** -0.5),
        "w_br_conv": nrm(ks[13], (L, D_CONV, D), D_CONV ** -0.5),
        "w_br_att": nrm(ks[14], (L, D_ATT, D), D_ATT ** -0.5),
        "w_out": nrm(ks[15], (L, D, D), BETA * D ** -0.5),
        "ln1_g": 1.0 + nrm(ks[16], (L, D), 0.02),
        "ln1_b": nrm(ks[17], (L, D), 0.02),
        "w_router": nrm(ks[18], (L, D, N_EXPERTS), D ** -0.5),
        "b_router": nrm(ks[19], (L, N_EXPERTS), 0.01),
        "w_gu": nrm(ks[20], (L, N_EXPERTS, D, 2 * D_FF), BETA * D ** -0.5),
        "b_gu": nrm(ks[21], (L, N_EXPERTS, 2 * D_FF), 0.02),
        "w_dn": nrm(ks[22], (L, N_EXPERTS, D_FF, D), BETA * D_FF ** -0.5),
        "b_dn": nrm(ks[23], (L, N_EXPERTS, D), 0.02),
        "ln2_g": 1.0 + nrm(ks[24], (L, D), 0.02),
        "ln2_b": nrm(ks[25], (L, D), 0.02),
    }


def reference(x_prompt, x_sample, cache_k, cache_v, cache_conv, c_prompt, c_sample,
              ln0_g, ln0_b, w_ada, b_ada, w_in, conv_w, w_br_conv, w_br_att, w_out,
              ln1_g, ln1_b, w_router, b_router, w_gu, b_gu, w_dn, b_dn, ln2_g, ln2_b):
    u_p = layer_norm(x_prompt, ln0_g, ln0_b)
    u_s = layer_norm(x_sample, ln0_g, ln0_b)
    bp = x_prompt.shape[0]
    kp_l, vp_l, cp_l, ks_l, vs_l, cs_l = [], [], [], [], [], []
    for l in range(DEPTH):
        layer_w = (w_ada[l], b_ada[l], w_in[l], conv_w[l], w_br_conv[l], w_br_att[l], w_out[l],
                   ln1_g[l], ln1_b[l], w_router[l], b_router[l], w_gu[l], b_gu[l], w_dn[l], b_dn[l],
                   ln2_g[l], ln2_b[l])
        conv0 = jnp.zeros((bp, CONV_WIDTH - 1, D_CONV), u_p.dtype)
        kv0 = jnp.zeros((bp, 0, SB_HEADS, SB_HEAD_DIM), u_p.dtype)
        u_p, k_p, v_p, cv_p = trunk_layer(u_p, c_prompt, conv0, kv0, kv0, *layer_w)
        u_s, k_s, v_s, cv_s = trunk_layer(u_s, c_sample, cache_conv[l], cache_k[l], cache_v[l], *layer_w)
        kp_l.append(k_p); vp_l.append(v_p); cp_l.append(cv_p)
        ks_l.append(k_s); vs_l.append(v_s); cs_l.append(cv_s)
    return (u_p, u_s, jnp.stack(kp_l), jnp.stack(vp_l), jnp.stack(cp_l),
            jnp.stack(ks_l), jnp.stack(vs_l), jnp.stack(cs_l))
```

```python
import numpy as np
import concourse.bass as bass
import concourse.mybir as mybir
from concourse.bass_utils import run_bass_kernel_spmd
from contextlib import ExitStack

F32 = mybir.dt.float32
BF16 = mybir.dt.bfloat16
I32 = mybir.dt.int32
U32 = mybir.dt.uint32
AF = mybir.ActivationFunctionType
ALU = mybir.AluOpType
AX = mybir.AxisListType


class Buf:
    __slots__ = ("ap", "w", "r", "name")

    def __init__(self, ap, name=""):
        self.ap = ap
        self.w = {}
        self.r = {}
        self.name = name

    def __getitem__(self, k):
        return self.ap[k]


class Eng:
    def __init__(self, fw, name, sem, dsems):
        self.fw = fw
        self.name = name
        self.sem = sem
        self.count = 0
        self.dsems = dsems
        self.rr = 0
        self.waited = {}
        self.prog = []


class FW:
    def __init__(self, nc, es, n_sp=24, n_pool=12, n_act=8):
        self.nc = nc
        self.es = es
        self.semid = {}
        def mk(name):
            s = es.enter_context(nc.semaphore(name))
            self.semid[id(s)] = s
            return s
        self.pe = Eng(self, "tensor", mk("s_pe"), [])
        self.act = Eng(self, "scalar", mk("s_act"), [[mk("d_act%d" % i), 0] for i in range(n_act)])
        self.dve = Eng(self, "vector", mk("s_dve"), [])
        self.pool = Eng(self, "gpsimd", mk("s_pool"), [[mk("d_pool%d" % i), 0] for i in range(n_pool)])
        self.sp = Eng(self, "sync", None, [[mk("d_sp%d" % i), 0] for i in range(n_sp)])
        self.engs = [self.pe, self.act, self.dve, self.pool, self.sp]
        self.n_inst = 0

    def _needs(self, E, reads, writes, skip_self=False):
        need = {}
        for b in reads:
            for s, v in b.w.items():
                if need.get(s, 0) < v:
                    need[s] = v
        for b in writes:
            for s, v in b.w.items():
                if need.get(s, 0) < v:
                    need[s] = v
            for s, v in b.r.items():
                if need.get(s, 0) < v:
                    need[s] = v
        waits = []
        for s, v in need.items():
            if skip_self and E.sem is not None and s is E.sem:
                continue
            if E.waited.get(id(s), 0) < v:
                E.waited[id(s)] = v
                waits.append((s, v))
        return waits

    def _record(self, tk_sem, tk_val, reads, writes):
        for b in reads:
            if b.r.get(tk_sem, 0) < tk_val:
                b.r[tk_sem] = tk_val
        for b in writes:
            b.w = {tk_sem: tk_val}
            b.r = {}

    def op(self, E, fn, reads=(), writes=(), skip_self=False, inc=True):
        waits = self._needs(E, reads, writes, skip_self=skip_self)
        self.n_inst += 1
        if inc:
            E.count += 1
            sem = E.sem
            def emit(e, waits=waits, fn=fn, sem=sem):
                for s, v in waits:
                    e.wait_ge(s, v)
                fn(e).then_inc(sem, 1)
            E.prog.append(emit)
            self._record(E.sem, E.count, reads, writes)
        else:
            def emit(e, waits=waits, fn=fn):
                for s, v in waits:
                    e.wait_ge(s, v)
                fn(e)
            E.prog.append(emit)

    def dma(self, Q, fn, reads=(), writes=()):
        d = Q.dsems[Q.rr]
        Q.rr = (Q.rr + 1) % len(Q.dsems)
        s = d[0]
        waits = []
        if d[1] > 0 and Q.waited.get(id(s), 0) < d[1]:
            Q.waited[id(s)] = d[1]
            waits.append((s, d[1]))
        waits += self._needs(Q, reads, writes)
        d[1] += 16
        val = d[1]
        self.n_inst += 1
        def emit(e, waits=waits, fn=fn, s=s):
            for ss, v in waits:
                e.wait_ge(ss, v)
            fn(e).then_inc(s, 16)
        Q.prog.append(emit)
        self._record(s, val, reads, writes)

    def finish(self):
        for Q in (self.sp, self.pool, self.act):
            for d in Q.dsems:
                if d[1] > 0:
                    for E in (self.sp,):
                        if E.waited.get(id(d[0]), 0) < d[1]:
                            E.waited[id(d[0])] = d[1]
                            E.prog.append(lambda e, s=d[0], v=d[1]: e.wait_ge(s, v))

    def barrier(self):
        tickets = []
        for E in (self.pe, self.act, self.dve, self.pool):
            if E.count > 0:
                tickets.append((E.sem, E.count))
        for Q in (self.sp, self.pool, self.act):
            for d in Q.dsems:
                if d[1] > 0:
                    tickets.append((d[0], d[1]))
        for E in self.engs:
            for s, v in tickets:
                if s is E.sem:
                    continue
                if E.waited.get(id(s), 0) < v:
                    E.waited[id(s)] = v
                    E.prog.append(lambda e, s=s, v=v: e.wait_ge(s, v))

    def emit_all(self):
        nc = self.nc
        progs = {E.name: E.prog for E in self.engs}
        for E in self.engs:
            E.prog = []
        self._emit(progs)

    def _emit(self, progs):
        nc = self.nc
        with nc.Block() as block:
            @block.tensor
            def _(e):
                for f in progs['tensor']:
                    f(e)
            @block.scalar
            def _(e):
                for f in progs['scalar']:
                    f(e)
            @block.vector
            def _(e):
                for f in progs['vector']:
                    f(e)
            @block.gpsimd
            def _(e):
                for f in progs['gpsimd']:
                    f(e)
            @block.sync
            def _(e):
                for f in progs['sync']:
                    f(e)

import math
D = 2048
KC = 16
NOWN = 2304
NPR = 2048
CAP = 512
NE = 32
ALPHA = 2.0 ** 0.25
QSCALE = 1.0 / math.sqrt(128.0)
EPS = 1e-5
PL = 2 + 2048 + 4 * 66
STOP_AFTER = 99

OWN_GROUPS = [(0, 512), (512, 512), (1024, 512), (1536, 512), (2048, 256)]


def segs_of(t0, n):
    if t0 < 2048:
        return [(0, n, 0)]
    return [(64 * s, 64, 1 + s) for s in range(4)]


def build_nc():
    nc = bass.Bass("TRN2", target_bir_lowering=False)

    def din(name, shape, dt=F32):
        return nc.dram_tensor(name, shape, dt, kind="ExternalInput").ap()

    def dout(name, shape, dt=F32):
        return nc.dram_tensor(name, shape, dt, kind="ExternalOutput").ap()

    def dint(name, shape, dt=F32):
        return nc.dram_tensor(name, shape, dt, kind="Internal").ap()

    xo = din("xo", [128, 16, NOWN]); xp = din("xp", [128, 16, NPR]); flag_d = din("flag", [128, 1])
    cT_d = din("cT", [128, 16, 8]); lnp_d = din("lnp", [128, 6, 16]); bada_d = din("bada", [128, 6, 16])
    wada = din("wada", [24, 128, 16, 512]); win = din("win", [80, 128, 16, 128])
    convw_d = din("convw", [128, 8, 3])
    wbc = din("wbc", [8, 128, 8, 256]); wba = din("wba", [8, 128, 8, 256]); wout = din("wout", [8, 128, 16, 256])
    wr_d = din("wr", [128, 16, 32]); br_d = din("br", [128, 32])
    NEW = 32 if STOP_AFTER >= 5 else 1
    wgu = din("wgu", [NEW, 16, 128, 16, 256]); bgu_d = din("bgu", [128, 32, 32])
    wdn = din("wdn", [NEW, 8, 128, 16, 256]); bdn_d = din("bdn", [32, 2048])
    ck = din("ck", [4, 8, 128, 2048]); cv = din("cv", [4, 8, 128, 16, 128]); cconv = din("cconv", [128, 8, 4, 2])

    yT = dout("yT", [128, 16, NOWN]); kTo = dout("kTo", [128, 8, NOWN]); vo = dout("vo", [NOWN, 1024])
    convo = dout("convo", [128, 8, 5, 2])

    uT_s = dint("uT_s", [128, 16, NOWN]); qT_s = dint("qT_s", [128, 8, NOWN], BF16)
    kT_s = dint("kT_s", [128, 8, NPR + NOWN], BF16); v_s = dint("v_s", [NPR + NOWN, 1024], BF16)
    ycT_s = dint("ycT_s", [128, 8, NOWN], BF16)
    sga_s = dint("sga_s", [128, 16, NOWN], BF16); sgb_s = dint("sgb_s", [128, 16, NOWN], BF16)
    oT_s = dint("oT_s", [128, 8, NOWN], BF16); u1T_s = dint("u1T_s", [128, 16, NOWN])
    xbuf = dint("xbuf", [NE * CAP, 2048], BF16); ybuf = dint("ybuf", [NE * CAP, 2048])

    dbufs = {}

    def db(*key):
        if key not in dbufs:
            dbufs[key] = Buf(None, str(key))
        return dbufs[key]

    with ExitStack() as es:
        fw = FW(nc, es)
        sp, act, dve, pool, pe = fw.sp, fw.act, fw.dve, fw.pool, fw.pe
        cnt = [0]

        def sbt(stack, name, shape, dt):
            cnt[0] += 1
            return Buf(stack.enter_context(nc.sbuf_tensor("%s_%d" % (name, cnt[0]), shape, dt)), name)

        banks = [Buf(es.enter_context(nc.psum_tensor("ps%d" % i, [128, 512], F32)), "ps%d" % i) for i in range(8)]
        bi = [0]

        def pbank():
            b = banks[bi[0] % 8]
            bi[0] += 1
            return b

        def ld(out_ap, in_ap, reads, writes, q=None):
            fw.dma(q or sp, lambda e: e.dma_start(out=out_ap, in_=in_ap), reads=reads, writes=writes)

        def mm_group(pb, out_ap, pairs, reads):
            n = len(pairs)
            for i, (l, r) in enumerate(pairs):
                fw.op(pe, lambda e, l=l, r=r, i=i: e.matmul(out_ap, lhsT=l, rhs=r, start=(i == 0), stop=(i == n - 1)),
                      reads=reads, writes=[pb], skip_self=True, inc=(i == n - 1))

        def tr_group(pb, items, reads):
            n = len(items)
            for i, (o, a, idn) in enumerate(items):
                fw.op(pe, lambda e, o=o, a=a, idn=idn: e.transpose(out=o, in_=a, identity=idn),
                      reads=reads, writes=[pb], skip_self=True, inc=(i == n - 1))

        ident = sbt(es, "ident", [128, 128], F32); identb = sbt(es, "identb", [128, 128], BF16)
        onesD = sbt(es, "onesD", [128, 128], F32); ones1 = sbt(es, "ones1", [128, 128], F32)
        ustr = sbt(es, "ustr", [128, 128], F32)
        maskL = sbt(es, "maskL", [128, 128], F32); maskLb = sbt(es, "maskLb", [128, 128], BF16)
        ones512 = sbt(es, "ones512", [128, 512], F32)
        iota32 = sbt(es, "iota32", [128, 32], F32)
        flag = sbt(es, "flag", [128, 1], F32)
        fw.op(pool, lambda e: e.memset(ident[:], 1.0), writes=[ident])
        fw.op(pool, lambda e: e.affine_select(out=ident[:], in_=ident[:], pattern=[[-1, 128]], compare_op=ALU.is_equal, fill=0.0, base=0, channel_multiplier=1), writes=[ident])
        fw.op(pool, lambda e: e.tensor_copy(out=identb[:], in_=ident[:]), reads=[ident], writes=[identb])
        fw.op(pool, lambda e: e.memset(onesD[:], 1.0 / D), writes=[onesD])
        fw.op(pool, lambda e: e.memset(ones1[:], 1.0), writes=[ones1])
        fw.op(pool, lambda e: e.memset(ones512[:], 1.0), writes=[ones512])
        fw.op(pool, lambda e: e.memset(maskL[:], 1.0), writes=[maskL])
        fw.op(pool, lambda e: e.affine_select(out=maskL[:], in_=maskL[:], pattern=[[-1, 128]], compare_op=ALU.is_gt, fill=0.0, base=0, channel_multiplier=1), writes=[maskL])
        fw.op(pool, lambda e: e.tensor_copy(out=maskLb[:], in_=maskL[:]), reads=[maskL], writes=[maskLb])
        fw.op(pool, lambda e: e.memset(ustr[:], 1.0), writes=[ustr])
        fw.op(pool, lambda e: e.affine_select(out=ustr[:], in_=ustr[:], pattern=[[1, 128]], compare_op=ALU.is_gt, fill=0.0, base=0, channel_multiplier=-1), writes=[ustr])
        fw.op(pool, lambda e: e.iota(iota32[:], pattern=[[1, 32]], base=0, channel_multiplier=0, allow_small_or_imprecise_dtypes=True), writes=[iota32])
        ld(flag[:], flag_d[:, :], [], [flag])

        cT = sbt(es, "cT", [128, 16, 8], F32); lnp = sbt(es, "lnp", [128, 6, 16], F32); bada = sbt(es, "bada", [128, 6, 16], F32)
        modT = sbt(es, "modT", [128, 6, 16, 8], F32)
        A0 = sbt(es, "A0", [128, 16, 8], F32); B0 = sbt(es, "B0", [128, 16, 8], F32)
        A2 = sbt(es, "A2", [128, 16, 8], F32); B2 = sbt(es, "B2", [128, 16, 8], F32)
        agb = sbt(es, "agb", [128, 4, 16], F32)
        convw = sbt(es, "convw", [128, 8, 3], F32)
        wr = sbt(es, "wr", [128, 16, 32], F32); brt = sbt(es, "brt", [128, 32], F32)
        rows_i = sbt(es, "rows_i", [128, 18, 4], I32); gates = sbt(es, "gates", [128, 18, 4], F32)
        combT = sbt(es, "combT", [32, NOWN], F32)
        cntb = sbt(es, "cntb", [128, 32], F32)
        hlast = sbt(es, "hlast", [128, 16, 2], BF16)
        for t, d_ in ((cT, cT_d), (lnp, lnp_d), (bada, bada_d), (convw, convw_d), (wr, wr_d)):
            ld(t[:], d_[:, :, :], [], [t])
        ld(brt[:], br_d[:, :], [], [brt])
        fw.op(pool, lambda e: e.memset(cntb[:], 0.0), writes=[cntb])

        def phase_end():
            fw.barrier()
            fw.emit_all()

        with ExitStack() as ph:
            wst = [sbt(ph, "wada_st", [128, 16, 512], F32) for _ in range(3)]
            modrow = sbt(ph, "modrow", [8, 12288], F32)
            fw.op(dve, lambda e: e.tensor_scalar_add(out=bada[:, 1:3, :], in0=bada[:, 1:3, :], scalar1=1.0), reads=[bada], writes=[bada])
            fw.op(dve, lambda e: e.tensor_scalar_add(out=bada[:, 4:6, :], in0=bada[:, 4:6, :], scalar1=1.0), reads=[bada], writes=[bada])
            for j in range(min(2, 24)):
                ld(wst[j % 3][:], wada[j, :, :, :], [], [wst[j % 3]])
            for j in range(24):
                if j + 2 < 24:
                    ld(wst[(j + 2) % 3][:], wada[j + 2, :, :, :], [], [wst[(j + 2) % 3]])
                w = wst[j % 3]
                pb = pbank()
                mm_group(pb, pb[0:8, :], [(cT[:, kc, :], w[:, kc, :]) for kc in range(16)], [w, cT])
                if j % 2 == 0:
                    fw.op(act, lambda e, pb=pb, j=j: e.copy(out=modrow[0:8, j * 512:(j + 1) * 512], in_=pb[0:8, :]), reads=[pb], writes=[modrow])
                else:
                    fw.op(dve, lambda e, pb=pb, j=j: e.tensor_copy(out=modrow[0:8, j * 512:(j + 1) * 512], in_=pb[0:8, :]), reads=[pb], writes=[modrow])
            for j4 in range(24):
                pb = pbank()
                tr_group(pb, [(pb[:, q * 8:(q + 1) * 8], modrow[0:8, (j4 * 4 + q) * 128:(j4 * 4 + q + 1) * 128], ident[0:8, 0:8]) for q in range(4)], [modrow, ident])
                for q in range(4):
                    jj = j4 * 4 + q
                    v, ch = jj // 16, jj % 16
                    fw.op(act, lambda e, pb=pb, v=v, ch=ch, q=q: e.activation(out=modT[:, v, ch, :], in_=pb[:, q * 8:(q + 1) * 8], func=AF.Identity, bias=bada[:, v, ch:ch + 1], scale=1.0),
                          reads=[pb, bada], writes=[modT])
            def bc3(ap2):
                return ap2.unsqueeze(2).to_broadcast([128, 16, 8])
            fw.op(dve, lambda e: e.tensor_tensor(out=A0[:], in0=modT[:, 1, :, :], in1=bc3(lnp[:, 0, :]), op=ALU.mult), reads=[modT, lnp], writes=[A0])
            fw.op(dve, lambda e: e.tensor_tensor(out=B0[:], in0=modT[:, 1, :, :], in1=bc3(lnp[:, 1, :]), op=ALU.mult), reads=[modT, lnp], writes=[B0])
            fw.op(dve, lambda e: e.tensor_tensor(out=B0[:], in0=B0[:], in1=modT[:, 0, :, :], op=ALU.add), reads=[modT], writes=[B0])
            fw.op(dve, lambda e: e.tensor_tensor(out=A2[:], in0=modT[:, 4, :, :], in1=bc3(lnp[:, 2, :]), op=ALU.mult), reads=[modT, lnp], writes=[A2])
            fw.op(dve, lambda e: e.tensor_tensor(out=B2[:], in0=modT[:, 4, :, :], in1=bc3(lnp[:, 3, :]), op=ALU.mult), reads=[modT, lnp], writes=[B2])
            fw.op(dve, lambda e: e.tensor_tensor(out=B2[:], in0=B2[:], in1=modT[:, 3, :, :], op=ALU.add), reads=[modT], writes=[B2])
            fw.op(dve, lambda e: e.tensor_scalar_mul(out=agb[:], in0=lnp[:, 0:4, :], scalar1=ALPHA), reads=[lnp], writes=[agb])
            phase_end()

        def ln_core(ph_tiles, src, n):
            sq, mean, msq, var, rstd = ph_tiles
            fw.op(act, lambda e: e.activation(out=sq[:, :, 0:n], in_=src[:, :, 0:n], func=AF.Square), reads=[src], writes=[sq])
            p1 = pbank(); p2 = pbank()
            mm_group(p1, p1[:, 0:n], [(onesD[:], src[:, kc, 0:n]) for kc in range(16)], [onesD, src])
            mm_group(p2, p2[:, 0:n], [(onesD[:], sq[:, kc, 0:n]) for kc in range(16)], [onesD, sq])
            fw.op(act, lambda e: e.copy(out=mean[:, 0:n], in_=p1[:, 0:n]), reads=[p1], writes=[mean])
            fw.op(dve, lambda e: e.tensor_tensor(out=msq[:, 0:n], in0=mean[:, 0:n], in1=mean[:, 0:n], op=ALU.mult), reads=[mean], writes=[msq])
            fw.op(dve, lambda e: e.tensor_tensor(out=var[:, 0:n], in0=p2[:, 0:n], in1=msq[:, 0:n], op=ALU.subtract), reads=[p2, msq], writes=[var])
            fw.op(dve, lambda e: e.tensor_scalar_add(out=var[:, 0:n], in0=var[:, 0:n], scalar1=EPS), reads=[var], writes=[var])
            fw.op(act, lambda e: e.activation(out=msq[:, 0:n], in_=var[:, 0:n], func=AF.Sqrt), reads=[var], writes=[msq])
            fw.op(dve, lambda e: e.reciprocal(out=rstd[:, 0:n], in_=msq[:, 0:n]), reads=[msq], writes=[rstd])
            fw.op(dve, lambda e: e.tensor_tensor(out=src[:, :, 0:n], in0=src[:, :, 0:n], in1=mean[:, 0:n].unsqueeze(1).to_broadcast([128, 16, n]), op=ALU.subtract), reads=[mean, sq], writes=[src])
            fw.op(dve, lambda e: e.tensor_tensor(out=src[:, :, 0:n], in0=src[:, :, 0:n], in1=rstd[:, 0:n].unsqueeze(1).to_broadcast([128, 16, n]), op=ALU.mult), reads=[rstd], writes=[src])

        def ln_tiles(ph):
            return (sbt(ph, "sq", [128, 16, 512], F32), sbt(ph, "mean", [128, 512], F32), sbt(ph, "msq", [128, 512], F32),
                    sbt(ph, "var", [128, 512], F32), sbt(ph, "rstd", [128, 512], F32))

        def affine(out_buf, out_fn, src, kc_scale_fn, kc_bias_fn, t0, n, extra_reads):
            for kc in range(16):
                for (o, l, s) in segs_of(t0, n):
                    fw.op(act, lambda e, kc=kc, o=o, l=l, s=s: e.activation(out=out_fn(kc, o, l), in_=src[:, kc, o:o + l], func=AF.Identity,
                                                                        bias=kc_bias_fn(kc, s), scale=kc_scale_fn(kc, s)),
                          reads=[src] + extra_reads, writes=[out_buf])

        class WS:
            def __init__(self, ph, kcmax, bw, nst=3, nwb=3, cast=None):
                self.cast = cast or (pool, dve)
                self.st = [sbt(ph, "wst", [128, kcmax, bw], F32) for _ in range(nst)]
                self.wlo = [sbt(ph, "wlo", [128, kcmax // 2, bw], BF16) for _ in range(nwb)]
                self.whi = [sbt(ph, "whi", [128, kcmax // 2, bw], BF16) for _ in range(nwb)]
                self.i = 0
                self.pending = []

            def issue(self, dram_ap, kc):
                st = self.st[self.i % len(self.st)]
                ld(st[:, 0:kc, :], dram_ap, [], [st])
                self.pending.append((st, kc, self.i))
                self.i += 1

            def get(self):
                st, kc, i = self.pending.pop(0)
                lo = self.wlo[i % len(self.wlo)]; hi = self.whi[i % len(self.whi)]
                h = kc // 2
                c0_, c1_ = self.cast
                if c0_ is act:
                    fw.op(act, lambda e: e.copy(out=lo[:, 0:h, :], in_=st[:, 0:h, :]), reads=[st], writes=[lo])
                else:
                    fw.op(c0_, lambda e: e.tensor_copy(out=lo[:, 0:h, :], in_=st[:, 0:h, :]), reads=[st], writes=[lo])
                fw.op(c1_, lambda e: e.tensor_copy(out=hi[:, 0:h, :], in_=st[:, h:kc, :]), reads=[st], writes=[hi])
                def w(k, a, b):
                    return lo[:, k, a:b] if k < h else hi[:, k - h, a:b]
                return w, [lo, hi]

        def stream(ws, blocks, body, depth=2):
            n = len(blocks)
            for i in range(min(depth + 1, n)):
                ws.issue(blocks[i][0], blocks[i][1])
            cur = ws.get() if n else None
            for i in range(n):
                if i + depth + 1 < n:
                    ws.issue(blocks[i + depth + 1][0], blocks[i + depth + 1][1])
                nxt = ws.get() if i + 1 < n else None
                body(cur[0], cur[1], blocks[i][2])
                cur = nxt

        with ExitStack() as ph12:
            hT = sbt(ph12, "hT", [128, 16, NOWN], BF16)

            def ln0_phase(xsrc, groups, store_u, tok_base):
                with ExitStack() as ph:
                    xg = [sbt(ph, "xg", [128, 16, 512], F32) for _ in range(1)]
                    lt = ln_tiles(ph)
                    sq = lt[0]
                    for gi, (t0, n) in enumerate(groups):
                        x = xg[0]
                        ld(x[:, :, 0:n], xsrc[:, :, t0:t0 + n], [], [x])
                        ln_core(lt, x, n)
                        affine(hT, lambda kc, o, l, t0=t0: hT[:, kc, t0 + o:t0 + o + l], x,
                               lambda kc, s: A0[:, kc, s:s + 1], lambda kc, s: B0[:, kc, s:s + 1], t0 + tok_base, n, [A0, B0])
                        if store_u:
                            affine(sq, lambda kc, o, l: sq[:, kc, o:o + l], x,
                                   lambda kc, s: agb[:, 0, kc:kc + 1], lambda kc, s: agb[:, 1, kc:kc + 1], t0 + tok_base, n, [agb])
                            ld(uT_s[:, :, t0:t0 + n], sq[:, :, 0:n], [sq], [db("uT", gi)])
                    phase_end()

            if STOP_AFTER >= 0:
                ln0_phase(xp, [(0, 512), (512, 512), (1024, 512), (1536, 512)], False, 0)
                fw.op(pool, lambda e: e.tensor_copy(out=hlast[:], in_=hT[:, :, 2046:2048]), reads=[hT], writes=[hlast])

            def proj_phase(ntok, groups, prev):
                with ExitStack() as ph:
                    ws = WS(ph, 16, 128)
                    ev32 = [sbt(ph, "ev32", [128, 512], F32) for _ in range(3)]
                    evb = [sbt(ph, "evb", [128, 512], BF16) for _ in range(3)]
                    ei = [0]
                    kbase = 0 if prev else NPR
                    if not prev:
                        xin = sbt(ph, "xin", [128, PL], F32); acc = sbt(ph, "acc", [128, PL], F32); ycb = sbt(ph, "ycb", [128, PL], BF16)
                        tmpc = sbt(ph, "tmpc", [128, 2], F32)

                    def pcol(t0):
                        return t0 + 2

                    def fm_matmul(w, wb, c0, t0, n):
                        pb = pbank()
                        mm_group(pb, pb[:, 0:n], [(w(kc, c0, c0 + 128), hT[:, kc, t0:t0 + n]) for kc in range(16)], wb + [hT])
                        return pb

                    def body(w, wb, info):
                        kind, idx = info
                        if kind in ("q", "k"):
                            for gi, (t0, n) in enumerate(groups):
                                pb = fm_matmul(w, wb, 0, t0, n)
                                b16 = evb[ei[0] % 3]; f32 = ev32[ei[0] % 3]; ei[0] += 1
                                if kind == "q":
                                    fw.op(act, lambda e, pb=pb, b16=b16, n=n: e.copy(out=b16[:, 0:n], in_=pb[:, 0:n]), reads=[pb], writes=[b16])
                                    ld(qT_s[:, idx, t0:t0 + n], b16[:, 0:n], [b16], [db("qT", idx, gi)])
                                else:
                                    fw.op(act, lambda e, pb=pb, f32=f32, n=n: e.copy(out=f32[:, 0:n], in_=pb[:, 0:n]), reads=[pb], writes=[f32])
                                    fw.op(dve, lambda e, f32=f32, b16=b16, n=n: e.tensor_copy(out=b16[:, 0:n], in_=f32[:, 0:n]), reads=[f32], writes=[b16])
                                    ld(kT_s[:, idx, kbase + t0:kbase + t0 + n], b16[:, 0:n], [b16], [db("kT", idx, prev, gi)])
                                    if not prev:
                                        ld(kTo[:, idx, t0:t0 + n], f32[:, 0:n], [f32], [db("kTo", idx, gi)])
                        elif kind == "v":
                            for tt in range(ntok // 128):
                                pb = pbank()
                                mm_group(pb, pb[:, 0:128], [(hT[:, kc, tt * 128:(tt + 1) * 128], w(kc, 0, 128)) for kc in range(16)], wb + [hT])
                                b16 = evb[ei[0] % 3]; f32 = ev32[ei[0] % 3]; ei[0] += 1
                                r0 = kbase + tt * 128
                                if prev:
                                    fw.op(dve, lambda e, pb=pb, b16=b16: e.tensor_scalar(out=b16[:, 0:128], in0=pb[:, 0:128], scalar1=flag[:, 0:1], scalar2=None, op0=ALU.mult), reads=[pb, flag], writes=[b16])
                                else:
                                    fw.op(act, lambda e, pb=pb, f32=f32: e.copy(out=f32[:, 0:128], in_=pb[:, 0:128]), reads=[pb], writes=[f32])
                                    fw.op(dve, lambda e, f32=f32, b16=b16: e.tensor_copy(out=b16[:, 0:128], in_=f32[:, 0:128]), reads=[f32], writes=[b16])
                                    ld(vo[tt * 128:(tt + 1) * 128, idx * 128:(idx + 1) * 128], f32[:, 0:128], [f32], [db("vo", idx, tt)])
                                ld(v_s[r0:r0 + 128, idx * 128:(idx + 1) * 128], b16[:, 0:128], [b16], [db("v_s", idx, prev, tt)])
                        elif kind in ("ga", "gb"):
                            dst = sga_s if kind == "ga" else sgb_s
                            for gi, (t0, n) in enumerate(groups):
                                pb = fm_matmul(w, wb, 0, t0, n)
                                b16 = evb[ei[0] % 3]; ei[0] += 1
                                fw.op(act, lambda e, pb=pb, b16=b16, n=n: e.activation(out=b16[:, 0:n], in_=pb[:, 0:n], func=AF.Sigmoid), reads=[pb], writes=[b16])
                                ld(dst[:, idx, t0:t0 + n], b16[:, 0:n], [b16], [db(kind, idx, gi)])
                        elif kind in ("xc", "cc", "bc"):
                            if kind == "xc":
                                ld(xin[:, 2050:2050 + 264].rearrange("p (s c) -> p s c", c=66)[:, :, 0:2], cconv[:, idx, :, :], [], [xin])
                                pbh = pbank()
                                mm_group(pbh, pbh[:, 0:2], [(w(kc, 0, 128), hlast[:, kc, :]) for kc in range(16)], wb + [hlast])
                                fw.op(act, lambda e, pbh=pbh: e.copy(out=xin[:, 0:2], in_=pbh[:, 0:2]), reads=[pbh], writes=[xin])
                            if kind == "cc":
                                pbh = pbank()
                                mm_group(pbh, pbh[:, 0:2], [(w(kc, 0, 128), hlast[:, kc, :]) for kc in range(16)], wb + [hlast])
                                fw.op(dve, lambda e, pbh=pbh: e.scalar_tensor_tensor(out=xin[:, 0:2], in0=pbh[:, 0:2], scalar=flag[:, 0:1], in1=xin[:, 0:2], op0=ALU.mult, op1=ALU.mult), reads=[pbh, flag], writes=[xin])
                            for gi, (t0, n) in enumerate(groups):
                                pb = fm_matmul(w, wb, 0, t0, n)
                                for (o, l, s) in segs_of(t0, n):
                                    c0 = (t0 + 2 + o) if s == 0 else (2052 + 66 * (s - 1))
                                    if kind == "xc":
                                        fw.op(act, lambda e, pb=pb, o=o, l=l, c0=c0: e.copy(out=xin[:, c0:c0 + l], in_=pb[:, o:o + l]), reads=[pb], writes=[xin])
                                    elif kind == "cc":
                                        fw.op(dve, lambda e, pb=pb, o=o, l=l, c0=c0: e.tensor_tensor(out=xin[:, c0:c0 + l], in0=pb[:, o:o + l], in1=xin[:, c0:c0 + l], op=ALU.mult), reads=[pb], writes=[xin])
                                    else:
                                        fw.op(dve, lambda e, pb=pb, o=o, l=l, c0=c0: e.tensor_tensor(out=ycb[:, c0:c0 + l], in0=pb[:, o:o + l], in1=acc[:, c0:c0 + l], op=ALU.mult), reads=[pb, acc], writes=[ycb])
                            if kind == "cc":
                                ld(convo[:, idx, 0, :], xin[:, 2048:2050], [xin], [db("convo", idx, 0)])
                                ld(convo[:, idx, 1:5, :], xin[:, 2050:2050 + 264].rearrange("p (s c) -> p s c", c=66)[:, :, 64:66], [xin], [db("convo", idx, 1)])
                                fw.op(pool, lambda e: e.tensor_scalar(out=acc[:, 2:PL], in0=xin[:, 0:PL - 2], scalar1=convw[:, idx, 0:1], scalar2=None, op0=ALU.mult), reads=[xin, convw], writes=[acc])
                                fw.op(dve, lambda e: e.scalar_tensor_tensor(out=acc[:, 2:PL], in0=xin[:, 1:PL - 1], scalar=convw[:, idx, 1:2], in1=acc[:, 2:PL], op0=ALU.mult, op1=ALU.add), reads=[xin, convw], writes=[acc])
                                fw.op(dve, lambda e: e.scalar_tensor_tensor(out=acc[:, 2:PL], in0=xin[:, 2:PL], scalar=convw[:, idx, 2:3], in1=acc[:, 2:PL], op0=ALU.mult, op1=ALU.add), reads=[xin, convw], writes=[acc])
                            if kind == "bc":
                                ld(ycT_s[:, idx, 0:2048], ycb[:, 2:2050], [ycb], [db("ycT", idx, 0)])
                                ld(ycT_s[:, idx, 2048:2304].rearrange("p (s c) -> p s c", c=64), ycb[:, 2050:2050 + 264].rearrange("p (s c) -> p s c", c=66)[:, :, 2:66], [ycb], [db("ycT", idx, 1)])

                    blocks = []
                    if prev:
                        import os
                        if os.environ.get("DBG", "kv").find("k") >= 0:
                          for i in range(8):
                            blocks.append((win[32 + i, :, :, :], 16, ("k", i)))
                        if os.environ.get("DBG", "kv").find("v") >= 0:
                          for i in range(8):
                            blocks.append((win[40 + i, :, :, :], 16, ("v", i)))
                    else:
                        for i in range(8):
                            blocks.append((win[i, :, :, :], 16, ("xc", i)))
                            blocks.append((win[16 + i, :, :, :], 16, ("cc", i)))
                            blocks.append((win[8 + i, :, :, :], 16, ("bc", i)))
                        for i in range(8):
                            blocks.append((win[24 + i, :, :, :], 16, ("q", i)))
                        for i in range(8):
                            blocks.append((win[32 + i, :, :, :], 16, ("k", i)))
                        for i in range(8):
                            blocks.append((win[40 + i, :, :, :], 16, ("v", i)))
                        for i in range(16):
                            blocks.append((win[48 + i, :, :, :], 16, ("ga", i)))
                        for i in range(16):
                            blocks.append((win[64 + i, :, :, :], 16, ("gb", i)))
                    stream(ws, blocks, body)
                    phase_end()

            if STOP_AFTER >= 1:
                proj_phase(NPR, [(0, 512), (512, 512), (1024, 512), (1536, 512)], True)
            if STOP_AFTER >= 2:
                ln0_phase(xo, OWN_GROUPS, True, 0)
                proj_phase(NOWN, OWN_GROUPS, False)

        if STOP_AFTER >= 3:
          with ExitStack() as ph:
            qh = [sbt(ph, "qh", [128, 2048], BF16) for _ in range(2)]
            kh = [sbt(ph, "kh", [128, 4096], BF16) for _ in range(2)]
            vh = [sbt(ph, "vh", [128, 32, 128], BF16) for _ in range(2)]
            oh = [sbt(ph, "oh", [128, 2048], BF16) for _ in range(2)]
            kc32 = [sbt(ph, "kc32", [128, 2048], F32) for _ in range(2)]
            vc32 = [sbt(ph, "vc32", [128, 16, 128], F32) for _ in range(2)]
            kcb = [sbt(ph, "kcb", [128, 2048], BF16) for _ in range(2)]
            vcb = [sbt(ph, "vcb", [128, 16, 128], BF16) for _ in range(2)]
            qs = [sbt(ph, "qs", [128, 64], BF16) for _ in range(2)]
            kn = [sbt(ph, "kn", [128, 64], BF16) for _ in range(2)]
            vn = [sbt(ph, "vn", [64, 128], BF16) for _ in range(2)]
            osm = [sbt(ph, "osm", [128, 64], BF16) for _ in range(2)]
            NR = 7
            eb = [sbt(ph, "eb", [128, 512], F32) for _ in range(NR)]
            spb = [sbt(ph, "spb", [128, 512], F32) for _ in range(NR)]
            csb = [sbt(ph, "csb", [128, 512], F32) for _ in range(NR)]
            ddb = [sbt(ph, "ddb", [128, 512], F32) for _ in range(NR)]
            wbb = [sbt(ph, "wbb", [128, 512], BF16) for _ in range(NR)]
            wTb = [sbt(ph, "wTb", [128, 512], BF16) for _ in range(NR)]
            ri = [0]

            ai = [0]
            b6 = [0]
            zi = [0]
            ti3 = [0]

            def pbank():
                b = banks[b6[0] % 6]
                b6[0] += 1
                return b

            jobs = []

            def attend(nq, q_ap, q_reads, chunks, out_fn, pre=None):
                st = {"po": None, "carry": None, "si": 0}
                total = sum(len(c[3]) for c in chunks)
                nch = len(chunks)
                for ci, (kT_ap, C, diag, vts, creads) in enumerate(chunks):
                    jb = {}

                    def A(jb=jb, ci=ci, kT_ap=kT_ap, C=C, diag=diag, creads=creads):
                        if ci == 0:
                            if pre is not None:
                                pre()
                        r = ri[0] % NR; ri[0] += 1
                        jb["r"] = r
                        pz = banks[zi[0] % 4]; zi[0] += 1
                        jb["pz"] = pz
                        mm_group(pz, pz[0:nq, 0:C], [(q_ap, kT_ap)], q_reads + creads)

                    def A1(jb=jb, ci=ci, C=C, diag=diag):
                        r = jb["r"]; pz = jb["pz"]
                        e_, s_ = eb[r], spb[r]
                        fw.op(act, lambda e: e.activation(out=e_[0:nq, 0:C], in_=pz[0:nq, 0:C], func=AF.Exp, scale=QSCALE), reads=[pz], writes=[e_])
                        fw.op(act, lambda e: e.activation(out=s_[0:nq, 0:C], in_=e_[0:nq, 0:C], func=AF.Ln, bias=1.0, scale=1.0), reads=[e_], writes=[s_])
                        if diag:
                            fw.op(pool, lambda e: e.tensor_tensor(out=s_[0:nq, 0:C], in0=s_[0:nq, 0:C], in1=maskL[0:nq, 0:C], op=ALU.mult), reads=[maskL], writes=[s_])

                    def B(jb=jb, ci=ci, kT_ap=kT_ap, C=C, diag=diag, vts=vts, creads=creads):
                        r = jb["r"]; pz = jb["pz"]
                        s_, cs, d_, w_, wT = spb[r], csb[r], ddb[r], wbb[r], wTb[r]
                        if ci == 0:
                            st["po"] = banks[6 + ai[0] % 2]
                            ai[0] += 1
                        po = st["po"]
                        carry = st["carry"]
                        if carry is None:
                            fw.op(dve, lambda e: e.tensor_tensor_scan(out=cs[0:nq, 0:C][:, ::-1], data0=ones512[0:nq, 0:C], data1=s_[0:nq, 0:C][:, ::-1], initial=0.0, op0=ALU.mult, op1=ALU.add),
                                  reads=[s_, ones512], writes=[cs])
                        else:
                            fw.op(dve, lambda e: e.tensor_tensor_scan(out=cs[0:nq, 0:C][:, ::-1], data0=ones512[0:nq, 0:C], data1=s_[0:nq, 0:C][:, ::-1], initial=carry[0:nq, 0:1], op0=ALU.mult, op1=ALU.add),
                                  reads=[s_, ones512, carry], writes=[cs])
                        st["carry"] = cs
                        e_ = eb[r]
                        fw.op(act, lambda e: e.activation(out=d_[0:nq, 0:C], in_=cs[0:nq, 0:C], func=AF.Exp, scale=-1.0), reads=[cs], writes=[d_])

                    def Cst(jb=jb, ci=ci, C=C, diag=diag, vts=vts):
                        r = jb["r"]
                        e_, d_, w_ = eb[r], ddb[r], wbb[r]
                        fw.op(pool, lambda e: e.tensor_tensor(out=w_[0:nq, 0:C], in0=e_[0:nq, 0:C], in1=d_[0:nq, 0:C], op=ALU.mult), reads=[e_, d_], writes=[w_])
                        if diag:
                            fw.op(pool, lambda e: e.tensor_tensor(out=w_[0:nq, 0:C], in0=w_[0:nq, 0:C], in1=maskLb[0:nq, 0:C], op=ALU.mult), reads=[maskLb], writes=[w_])
                        nsub = len(vts)
                        pt = banks[4 + ti3[0] % 2]; ti3[0] += 1
                        jb["pt"] = pt
                        ptb = pt[:].bitcast(BF16)
                        items = []
                        for j, (v_ap, K) in enumerate(vts):
                            items.append((ptb[0:K, j * 128:j * 128 + nq], w_[0:nq, j * 128:j * 128 + K], identb[0:nq, 0:nq]))
                        tr_group(pt, items, [w_, identb])

                    def B2(jb=jb, ci=ci, C=C, vts=vts, creads=creads):
                        r = jb["r"]; pt = jb["pt"]
                        wT = wTb[r]
                        po = st["po"]
                        nsub = len(vts)
                        ptb = pt[:].bitcast(BF16)
                        Kmax = max(K for _, K in vts)
                        src = ptb[0:Kmax, 0:nsub * 128].rearrange("p (j c) -> p j c", c=128)[:, :, 0:nq]
                        dst = wT[0:Kmax, 0:nsub * 128].rearrange("p (j c) -> p j c", c=128)[:, :, 0:nq]
                        ri[0] += 0
                        fw.op(dve, lambda e: e.tensor_copy(out=dst, in_=src), reads=[pt], writes=[wT])
                        for j, (v_ap, K) in enumerate(vts):
                            si = st["si"]
                            fw.op(pe, lambda e, v_ap=v_ap, K=K, j=j, si=si: e.matmul(po[:, 0:nq], lhsT=v_ap, rhs=wT[0:K, j * 128:j * 128 + nq], start=(si == 0), stop=(si == total - 1)),
                                  reads=[wT] + creads, writes=[po], skip_self=True, inc=(j == nsub - 1))
                            st["si"] += 1
                        if ci == nch - 1:
                            out_fn(po)

                    jobs.append((A, A1, B, Cst, B2))

            def run_jobs(la=2):
                n = len(jobs)
                for i in range(-1, n + la + 2):
                    if 0 <= i + 1 < n:
                        jobs[i + 1][0]()
                    if 0 <= i < n:
                        jobs[i][1]()
                    if 0 <= i - la < n:
                        jobs[i - la][2]()
                    if 0 <= i - la - 1 < n:
                        jobs[i - la - 1][3]()
                    if 0 <= i - la - 2 < n:
                        jobs[i - la - 2][4]()
                del jobs[:]

            for h in range(8):
                q_, k_, v_, o_ = qh[h % 2], kh[h % 2], vh[h % 2], oh[h % 2]

                def pre_p(h=h, q_=q_, k_=k_, v_=v_):
                    ld(q_[:], qT_s[:, h, 0:2048], [db("qT", h, g) for g in range(4)], [q_])
                    ld(k_[:], kT_s[:, h, 0:4096], [db("kT", h, True, g) for g in range(4)] + [db("kT", h, False, g) for g in range(4)], [k_])
                    ld(v_[:], v_s[0:4096, h * 128:(h + 1) * 128].rearrange("(t p) d -> p t d", p=128),
                       [db("v_s", h, True, t) for t in range(16)] + [db("v_s", h, False, t) for t in range(16)], [v_])
                for qi in range(16):
                    chunks = []
                    d0 = NPR + qi * 128
                    chunks.append((k_[:, d0:d0 + 128], 128, True, [(v_[:, 16 + qi, :], 128)], [k_, v_]))
                    L = d0
                    for st in reversed(range(0, L, 512)):
                        C = min(512, L - st)
                        chunks.append((k_[:, st:st + C], C, False, [(v_[:, st // 128 + j, :], 128) for j in range(C // 128)], [k_, v_]))
                    def outp(po, qi=qi, o_=o_, h=h):
                        fw.op(act, lambda e: e.copy(out=o_[:, qi * 128:(qi + 1) * 128], in_=po[:, 0:128]), reads=[po], writes=[o_])
                        if qi == 15:
                            ld(oT_s[:, h, 0:2048], o_[:], [o_], [db("oT", h, 0)])
                    attend(128, q_[:, qi * 128:(qi + 1) * 128], [q_], chunks, outp, pre=(pre_p if qi == 0 else None))

            ui = 0
            for s in range(4):
                for h in range(8):
                    b = ui % 2; ui += 1
                    t0 = 2048 + 64 * s

                    def pre_s(b=b, s=s, h=h, t0=t0):
                        ld(kc32[b][:], ck[s, h, :, :], [], [kc32[b]])
                        ld(vc32[b][:], cv[s, h, :, :, :], [], [vc32[b]])
                        ld(qs[b][:], qT_s[:, h, t0:t0 + 64], [db("qT", h, 4)], [qs[b]])
                        ld(kn[b][:], kT_s[:, h, NPR + t0:NPR + t0 + 64], [db("kT", h, False, 4)], [kn[b]])
                        ld(vn[b][:], v_s[NPR + t0:NPR + t0 + 64, h * 128:(h + 1) * 128], [db("v_s", h, False, 16 + s // 2)], [vn[b]])
                        fw.op(pool, lambda e: e.tensor_copy(out=kcb[b][:], in_=kc32[b][:]), reads=[kc32[b]], writes=[kcb[b]])
                        fw.op(pool, lambda e: e.tensor_copy(out=vcb[b][:], in_=vc32[b][:]), reads=[vc32[b]], writes=[vcb[b]])
                    chunks = [(kn[b][:, 0:64], 64, True, [(vn[b][0:64, :], 64)], [kn[b], vn[b]])]
                    for st in (1536, 1024, 512, 0):
                        chunks.append((kcb[b][:, st:st + 512], 512, False, [(vcb[b][:, st // 128 + j, :], 128) for j in range(4)], [kcb[b], vcb[b]]))
                    def outp(po, b=b, h=h, t0=t0):
                        fw.op(act, lambda e: e.copy(out=osm[b][:], in_=po[:, 0:64]), reads=[po], writes=[osm[b]])
                        ld(oT_s[:, h, t0:t0 + 64], osm[b][:], [osm[b]], [db("oT", h, 1 + (t0 - 2048) // 64)])
                    attend(64, qs[b][:], [qs[b]], chunks, outp, pre=pre_s)
            run_jobs(2)
            phase_end()

        if STOP_AFTER >= 4:
          with ExitStack() as ph:
            ws = WS(ph, 16, 256, nst=2, nwb=3)
            ycg = sbt(ph, "ycg", [128, 8, 512], BF16); og = sbt(ph, "og", [128, 8, 512], BF16)
            mT = sbt(ph, "mT", [128, 16, 512], BF16); rT = sbt(ph, "rT", [128, 16, 512], F32)
            lt = ln_tiles(ph); sq = lt[0]
            sga_t = [sbt(ph, "sga_t", [128, 512], BF16) for _ in range(2)]
            sgb_t = [sbt(ph, "sgb_t", [128, 512], BF16) for _ in range(2)]
            au_t = [sbt(ph, "au_t", [128, 512], F32) for _ in range(2)]
            m1 = sbt(ph, "m1", [128, 512], F32); m2 = sbt(ph, "m2", [128, 512], F32)
            h2tok = [sbt(ph, "h2tok", [128, 2048], BF16) for _ in range(1)]
            lg = sbt(ph, "lg", [128, 32], F32); mx8 = sbt(ph, "mx8", [128, 8], F32); idx8 = sbt(ph, "idx8", [128, 8], U32)
            negmax = sbt(ph, "negmax", [128, 1], F32); ek = sbt(ph, "ek", [128, 4], F32); ssum = sbt(ph, "ssum", [128, 1], F32)
            rs = sbt(ph, "rs", [128, 1], F32); Mk = sbt(ph, "Mk", [128, 32], F32); pos = sbt(ph, "pos", [128, 32], F32)
            ohk = sbt(ph, "ohk", [128, 32], F32); tmp = sbt(ph, "tmp", [128, 32], F32); posk = sbt(ph, "posk", [128, 4], F32)
            ekf = sbt(ph, "ekf", [128, 4], F32); rowf = sbt(ph, "rowf", [128, 4], F32); comb = sbt(ph, "comb", [128, 32], F32)
            ti = [0]
            for gi, (t0, n) in enumerate(OWN_GROUPS):
                ld(ycg[:, :, 0:n], ycT_s[:, :, t0:t0 + n], [db("ycT", i, 0 if t0 < 2048 else 1) for i in range(8)], [ycg])
                ld(og[:, :, 0:n], oT_s[:, :, t0:t0 + n], [db("oT", hh, 0) for hh in range(8)] if t0 < 2048 else [db("oT", hh, 1 + s_) for hh in range(8) for s_ in range(4)], [og])
                held = {}

                def body1(w, wb, info, t0=t0, n=n, gi=gi):
                    kind, j = info
                    if kind == "c":
                        held["c"] = (w, wb)
                        return
                    wc, wcb = held["c"]
                    for half in range(2):
                        ft = 2 * j + half
                        r = ti[0] % 2; ti[0] += 1
                        ld(sga_t[r][:, 0:n], sga_s[:, ft, t0:t0 + n], [db("ga", ft, gi)], [sga_t[r]])
                        ld(sgb_t[r][:, 0:n], sgb_s[:, ft, t0:t0 + n], [db("gb", ft, gi)], [sgb_t[r]])
                        pc = pbank(); pa = pbank()
                        mm_group(pc, pc[:, 0:n], [(wc(kc, half * 128, half * 128 + 128), ycg[:, kc, 0:n]) for kc in range(8)], wcb + [ycg])
                        mm_group(pa, pa[:, 0:n], [(w(kc, half * 128, half * 128 + 128), og[:, kc, 0:n]) for kc in range(8)], wb + [og])
                        fw.op(dve, lambda e, pc=pc, r=r: e.tensor_tensor(out=m1[:, 0:n], in0=pc[:, 0:n], in1=sga_t[r][:, 0:n], op=ALU.mult), reads=[pc, sga_t[r]], writes=[m1])
                        fw.op(dve, lambda e, pa=pa, r=r: e.tensor_tensor(out=m2[:, 0:n], in0=pa[:, 0:n], in1=sgb_t[r][:, 0:n], op=ALU.mult), reads=[pa, sgb_t[r]], writes=[m2])
                        fw.op(pool, lambda e, ft=ft: e.tensor_tensor(out=mT[:, ft, 0:n], in0=m1[:, 0:n], in1=m2[:, 0:n], op=ALU.add), reads=[m1, m2], writes=[mT])

                blocks = []
                for j in range(8):
                    blocks.append((wbc[j, :, :, :], 8, ("c", j)))
                    blocks.append((wba[j, :, :, :], 8, ("a", j)))
                stream(ws, blocks, body1, depth=1)

                def body2(w, wb, info, t0=t0, n=n, gi=gi):
                    j = info
                    for half in range(2):
                        ft = 2 * j + half
                        r = ti[0] % 2; ti[0] += 1
                        ld(au_t[r][:, 0:n], uT_s[:, ft, t0:t0 + n], [db("uT", gi)], [au_t[r]])
                        pm = pbank()
                        mm_group(pm, pm[:, 0:n], [(w(kc, half * 128, half * 128 + 128), mT[:, kc, 0:n]) for kc in range(16)], wb + [mT])
                        for (o, l, s) in segs_of(t0, n):
                            fw.op(dve, lambda e, pm=pm, r=r, ft=ft, o=o, l=l, s=s: e.scalar_tensor_tensor(out=rT[:, ft, o:o + l], in0=pm[:, o:o + l], scalar=modT[:, 2, ft, s:s + 1], in1=au_t[r][:, o:o + l], op0=ALU.mult, op1=ALU.add),
                                  reads=[pm, modT, au_t[r]], writes=[rT])

                stream(ws, [(wout[j, :, :, :], 16, j) for j in range(8)], body2, depth=1)
                ln_core(lt, rT, n)
                affine(sq, lambda kc, o, l: sq[:, kc, o:o + l], rT, lambda kc, s: agb[:, 2, kc:kc + 1], lambda kc, s: agb[:, 3, kc:kc + 1], t0, n, [agb])
                ld(u1T_s[:, :, t0:t0 + n], sq[:, :, 0:n], [sq], [db("u1T", gi)])
                affine(rT, lambda kc, o, l: rT[:, kc, o:o + l], rT, lambda kc, s: A2[:, kc, s:s + 1], lambda kc, s: B2[:, kc, s:s + 1], t0, n, [A2, B2])
                for tt in range(n // 128):
                    T = (t0 + tt * 128) // 128
                    c0 = tt * 128
                    pl = pbank()
                    mm_group(pl, pl[:, 0:32], [(rT[:, kc, c0:c0 + 128], wr[:, kc, :]) for kc in range(16)], [rT, wr])
                    fw.op(dve, lambda e, pl=pl: e.tensor_tensor(out=lg[:], in0=pl[:, 0:32], in1=brt[:], op=ALU.add), reads=[pl, brt], writes=[lg])
                    fw.op(dve, lambda e: e.max(out=mx8[:], in_=lg[:]), reads=[lg], writes=[mx8])
                    fw.op(dve, lambda e: e.max_index(out=idx8[:], in_max=mx8[:], in_values=lg[:]), reads=[lg, mx8], writes=[idx8])
                    fw.op(dve, lambda e: e.tensor_scalar_mul(out=negmax[:], in0=mx8[:, 0:1], scalar1=-1.0), reads=[mx8], writes=[negmax])
                    fw.op(act, lambda e: e.activation(out=ek[:], in_=mx8[:, 0:4], func=AF.Exp, bias=negmax[:, 0:1], scale=1.0, accum_out=ssum[:, 0:1]), reads=[mx8, negmax], writes=[ek, ssum])
                    fw.op(dve, lambda e: e.reciprocal(out=rs[:], in_=ssum[:]), reads=[ssum], writes=[rs])
                    fw.op(dve, lambda e, T=T: e.tensor_scalar_mul(out=gates[:, T, :], in0=ek[:], scalar1=rs[:, 0:1]), reads=[ek, rs], writes=[gates])
                    fw.op(dve, lambda e: e.tensor_scalar(out=Mk[:], in0=lg[:], scalar1=mx8[:, 3:4], scalar2=None, op0=ALU.is_ge), reads=[lg, mx8], writes=[Mk])
                    pp = pbank()
                    mm_group(pp, pp[:, 0:32], [(ustr[:], Mk[:])], [ustr, Mk])
                    fw.op(dve, lambda e, pp=pp: e.tensor_tensor(out=pos[:], in0=pp[:, 0:32], in1=cntb[:], op=ALU.add), reads=[pp, cntb], writes=[pos])
                    pc2 = pbank()
                    mm_group(pc2, pc2[:, 0:32], [(ones1[:], Mk[:])], [ones1, Mk])
                    fw.op(dve, lambda e, pc2=pc2: e.tensor_tensor(out=cntb[:], in0=pc2[:, 0:32], in1=cntb[:], op=ALU.add), reads=[pc2], writes=[cntb])
                    fw.op(dve, lambda e: e.tensor_copy(out=ekf[:], in_=idx8[:, 0:4]), reads=[idx8], writes=[ekf])
                    for k in range(4):
                        fw.op(dve, lambda e, k=k: e.tensor_scalar(out=ohk[:], in0=iota32[:], scalar1=ekf[:, k:k + 1], scalar2=None, op0=ALU.is_equal), reads=[iota32, ekf], writes=[ohk])
                        fw.op(dve, lambda e: e.tensor_tensor(out=tmp[:], in0=ohk[:], in1=pos[:], op=ALU.mult), reads=[ohk, pos], writes=[tmp])
                        fw.op(dve, lambda e, k=k: e.reduce_sum(out=posk[:, k:k + 1], in_=tmp[:], axis=AX.X), reads=[tmp], writes=[posk])
                        if k == 0:
                            fw.op(dve, lambda e, T=T: e.tensor_scalar_mul(out=comb[:], in0=ohk[:], scalar1=gates[:, T, 0:1]), reads=[ohk, gates], writes=[comb])
                        else:
                            fw.op(dve, lambda e, T=T, k=k: e.scalar_tensor_tensor(out=comb[:], in0=ohk[:], scalar=gates[:, T, k:k + 1], in1=comb[:], op0=ALU.mult, op1=ALU.add), reads=[ohk, gates], writes=[comb])
                    fw.op(dve, lambda e: e.scalar_tensor_tensor(out=rowf[:], in0=ekf[:], scalar=float(CAP), in1=posk[:], op0=ALU.mult, op1=ALU.add), reads=[ekf, posk], writes=[rowf])
                    fw.op(dve, lambda e, T=T: e.tensor_copy(out=rows_i[:, T, :], in_=rowf[:]), reads=[rowf], writes=[rows_i])
                    ptc = pbank()
                    tr_group(ptc, [(ptc[0:32, 0:128], comb[:], ident[:])], [comb, ident])
                    fw.op(act, lambda e, ptc=ptc, T=T: e.copy(out=combT[:, T * 128:(T + 1) * 128], in_=ptc[0:32, 0:128]), reads=[ptc], writes=[combT])
                    hb = h2tok[0]
                    for q4 in range(4):
                        pt = pbank()
                        tr_group(pt, [(pt[:, j * 128:(j + 1) * 128], rT[:, q4 * 4 + j, c0:c0 + 128], ident[:]) for j in range(4)], [rT, ident])
                        if q4 % 2 == 0:
                            fw.op(act, lambda e, pt=pt, q4=q4, hb=hb: e.copy(out=hb[:, q4 * 512:(q4 + 1) * 512], in_=pt[:, :]), reads=[pt], writes=[hb])
                        else:
                            fw.op(dve, lambda e, pt=pt, q4=q4, hb=hb: e.tensor_copy(out=hb[:, q4 * 512:(q4 + 1) * 512], in_=pt[:, :]), reads=[pt], writes=[hb])
                    for k in range(4):
                        fw.dma(pool, lambda e, T=T, k=k, hb=hb: e.indirect_dma_start(out=xbuf[:, :], out_offset=bass.IndirectOffsetOnAxis(ap=rows_i[:, T, k:k + 1], axis=0), in_=hb[:], in_offset=None),
                               reads=[hb, rows_i], writes=[db("xbuf")])
            phase_end()

        if STOP_AFTER >= 5:
          with ExitStack() as ph:
            ws = WS(ph, 16, 256, nst=3, nwb=2, cast=(act, dve))
            bgu = sbt(ph, "bgu", [128, 32, 32], F32)
            ld(bgu[:], bgu_d[:, :, :], [], [bgu])
            Xe = sbt(ph, "Xe", [128, 4, 2048], BF16); XeT = sbt(ph, "XeT", [128, 16, 512], BF16)
            gs = sbt(ph, "gs", [128, 16, 512], BF16); actT = sbt(ph, "actT", [128, 16, 512], BF16)
            yt = sbt(ph, "yt", [128, 4, 2048], F32)
            gtmp = [sbt(ph, "gtmp", [128, 512], F32) for _ in range(2)]
            sgt = [sbt(ph, "sgt", [128, 512], F32) for _ in range(2)]
            ti = [0]

            def body(w, wb, info):
                e_, kind, j = info
                if kind == "g" and j == 0:
                    ld(Xe[:], xbuf[e_ * CAP:(e_ + 1) * CAP, :].rearrange("(t p) d -> p t d", p=128), [db("xbuf")], [Xe])
                    for kc in range(16):
                        pt = pbank(); ptb = pt[:].bitcast(BF16)
                        tr_group(pt, [(ptb[:, tt * 128:(tt + 1) * 128], Xe[:, tt, kc * 128:(kc + 1) * 128], identb[:]) for tt in range(4)], [Xe, identb])
                        if kc % 2 == 0:
                            fw.op(act, lambda e, ptb=ptb, kc=kc: e.copy(out=XeT[:, kc, :], in_=ptb[:, 0:512]), reads=[pt], writes=[XeT])
                        else:
                            fw.op(dve, lambda e, ptb=ptb, kc=kc: e.tensor_copy(out=XeT[:, kc, :], in_=ptb[:, 0:512]), reads=[pt], writes=[XeT])
                if kind in ("g", "u"):
                    for half in range(2):
                        ft = 2 * j + half
                        r = ti[0] % 2; ti[0] += 1
                        pg = pbank()
                        mm_group(pg, pg[:, :], [(w(kc, half * 128, half * 128 + 128), XeT[:, kc, :]) for kc in range(16)], wb + [XeT])
                        g_ = gtmp[r]; s_ = sgt[r]
                        if kind == "g":
                            fw.op(dve, lambda e, pg=pg, g_=g_, ft=ft: e.tensor_scalar(out=g_[:], in0=pg[:, :], scalar1=bgu[:, e_, ft:ft + 1], scalar2=7.0, op0=ALU.add, op1=ALU.min), reads=[pg, bgu], writes=[g_])
                            fw.op(act, lambda e, g_=g_, s_=s_: e.activation(out=s_[:], in_=g_[:], func=AF.Sigmoid, scale=1.702), reads=[g_], writes=[s_])
                            fw.op(pool, lambda e, g_=g_, s_=s_, ft=ft: e.tensor_tensor(out=gs[:, ft, :], in0=g_[:], in1=s_[:], op=ALU.mult), reads=[g_, s_], writes=[gs])
                        else:
                            fw.op(dve, lambda e, pg=pg, g_=g_, ft=ft: e.tensor_scalar(out=g_[:], in0=pg[:, :], scalar1=bgu[:, e_, 16 + ft:17 + ft], scalar2=7.0, op0=ALU.add, op1=ALU.min), reads=[pg, bgu], writes=[g_])
                            fw.op(dve, lambda e, g_=g_, s_=s_: e.tensor_scalar(out=s_[:], in0=g_[:], scalar1=-7.0, scalar2=1.0, op0=ALU.max, op1=ALU.add), reads=[g_], writes=[s_])
                            fw.op(pool, lambda e, s_=s_, ft=ft: e.tensor_tensor(out=actT[:, ft, :], in0=s_[:], in1=gs[:, ft, :], op=ALU.mult), reads=[s_, gs], writes=[actT])
                else:
                    for tt in range(4):
                        py = pbank()
                        mm_group(py, py[:, 0:256], [(actT[:, kc, tt * 128:(tt + 1) * 128], w(kc, 0, 256)) for kc in range(16)], wb + [actT])
                        if tt % 2 == 0:
                            fw.op(act, lambda e, py=py, tt=tt: e.copy(out=yt[:, tt, j * 256:(j + 1) * 256], in_=py[:, 0:256]), reads=[py], writes=[yt])
                        else:
                            fw.op(dve, lambda e, py=py, tt=tt: e.tensor_copy(out=yt[:, tt, j * 256:(j + 1) * 256], in_=py[:, 0:256]), reads=[py], writes=[yt])
                    if j == 7:
                        ld(ybuf[e_ * CAP:(e_ + 1) * CAP, :].rearrange("(t p) d -> p t d", p=128), yt[:], [yt], [db("ybuf", e_)])

            blocks = []
            for e_ in range(NE):
                for j in range(8):
                    blocks.append((wgu[e_, j, :, :, :], 16, (e_, "g", j)))
                for j in range(8):
                    blocks.append((wgu[e_, 8 + j, :, :, :], 16, (e_, "u", j)))
                for j in range(8):
                    blocks.append((wdn[e_, j, :, :, :], 16, (e_, "d", j)))
            stream(ws, blocks, body, depth=2)
            phase_end()

        if STOP_AFTER >= 6:
          with ExitStack() as ph:
            bdn = sbt(ph, "bdn", [32, 2048], F32)
            ld(bdn[:], bdn_d[:, :], [], [bdn])
            yk = [sbt(ph, "yk", [128, 2048], F32) for _ in range(4)]
            f_ = sbt(ph, "f_", [128, 2048], F32); r2 = sbt(ph, "r2", [128, 16, 512], F32)
            au1 = sbt(ph, "au1", [128, 16, 512], F32)
            lt = ln_tiles(ph)
            allyb = [db("ybuf", e_) for e_ in range(NE)]
            for gi, (t0, n) in enumerate(OWN_GROUPS):
                ld(au1[:, :, 0:n], u1T_s[:, :, t0:t0 + n], [db("u1T", gi)], [au1])
                for tt in range(n // 128):
                    T = (t0 + tt * 128) // 128
                    c0 = tt * 128
                    for k in range(4):
                        fw.dma(pool, lambda e, T=T, k=k: e.indirect_dma_start(out=yk[k][:], out_offset=None, in_=ybuf[:, :], in_offset=bass.IndirectOffsetOnAxis(ap=rows_i[:, T, k:k + 1], axis=0)),
                               reads=allyb + [rows_i], writes=[yk[k]])
                    for q4 in range(4):
                        pb = pbank()
                        mm_group(pb, pb[:, :], [(combT[0:32, T * 128:(T + 1) * 128], bdn[0:32, q4 * 512:(q4 + 1) * 512])], [combT, bdn])
                        fw.op(dve, lambda e, pb=pb, q4=q4, T=T: e.scalar_tensor_tensor(out=f_[:, q4 * 512:(q4 + 1) * 512], in0=yk[0][:, q4 * 512:(q4 + 1) * 512], scalar=gates[:, T, 0:1], in1=pb[:, :], op0=ALU.mult, op1=ALU.add),
                              reads=[pb, yk[0], gates], writes=[f_])
                    for k in range(1, 4):
                        eng = dve
                        fw.op(eng, lambda e, k=k, T=T: e.scalar_tensor_tensor(out=f_[:], in0=yk[k][:], scalar=gates[:, T, k:k + 1], in1=f_[:], op0=ALU.mult, op1=ALU.add), reads=[yk[k], gates], writes=[f_])
                    for q4 in range(4):
                        pt = pbank()
                        tr_group(pt, [(pt[:, j * 128:(j + 1) * 128], f_[:, (q4 * 4 + j) * 128:(q4 * 4 + j + 1) * 128], ident[:]) for j in range(4)], [f_, ident])
                        for j in range(4):
                            kc = q4 * 4 + j
                            for (o, l, s) in segs_of(t0, n):
                                lo_ = max(o, c0); hi_ = min(o + l, c0 + 128)
                                if lo_ >= hi_:
                                    continue
                                fw.op(dve, lambda e, pt=pt, j=j, kc=kc, lo_=lo_, hi_=hi_, s=s, c0=c0: e.scalar_tensor_tensor(out=r2[:, kc, lo_:hi_], in0=pt[:, j * 128 + lo_ - c0:j * 128 + hi_ - c0], scalar=modT[:, 5, kc, s:s + 1], in1=au1[:, kc, lo_:hi_], op0=ALU.mult, op1=ALU.add),
                                      reads=[pt, modT, au1], writes=[r2])
                ln_core(lt, r2, n)
                affine(r2, lambda kc, o, l: r2[:, kc, o:o + l], r2, lambda kc, s: lnp[:, 4, kc:kc + 1], lambda kc, s: lnp[:, 5, kc:kc + 1], t0, n, [lnp])
                ld(yT[:, :, t0:t0 + n], r2[:, :, 0:n], [r2], [db("yT", gi)])
            phase_end()

        fw.finish()
        fw.emit_all()
    return nc


def _prep_shared(inp):
    f = lambda a: np.ascontiguousarray(a, dtype=np.float32)
    sh = {}
    sh["lnp"] = f(np.stack([inp["ln0_g"], inp["ln0_b"], inp["ln1_g"][0], inp["ln1_b"][0], inp["ln2_g"][0], inp["ln2_b"][0]]).reshape(6, 16, 128).transpose(2, 0, 1))
    sh["bada"] = f(np.asarray(inp["b_ada"])[0].reshape(6, 16, 128).transpose(2, 0, 1))
    sh["wada"] = f(np.asarray(inp["w_ada"])[0].reshape(16, 128, 24, 512).transpose(2, 1, 0, 3))
    sh["win"] = f(np.asarray(inp["w_in"])[0].reshape(16, 128, 80, 128).transpose(2, 1, 0, 3))
    sh["convw"] = f(np.asarray(inp["conv_w"])[0].reshape(3, 8, 128).transpose(2, 1, 0))
    sh["wbc"] = f(np.asarray(inp["w_br_conv"])[0].reshape(8, 128, 8, 256).transpose(2, 1, 0, 3))
    sh["wba"] = f(np.asarray(inp["w_br_att"])[0].reshape(8, 128, 8, 256).transpose(2, 1, 0, 3))
    sh["wout"] = f(np.asarray(inp["w_out"])[0].reshape(16, 128, 8, 256).transpose(2, 1, 0, 3))
    sh["wr"] = f(np.asarray(inp["w_router"])[0].reshape(16, 128, 32).transpose(1, 0, 2))
    sh["br"] = f(np.tile(np.asarray(inp["b_router"])[0][None, :], (128, 1)))
    sh["wgu"] = f(np.asarray(inp["w_gu"])[0].reshape(32, 16, 128, 16, 256).transpose(0, 3, 2, 1, 4))
    sh["bgu"] = f(np.asarray(inp["b_gu"])[0].reshape(32, 32, 128).transpose(2, 0, 1))
    sh["wdn"] = f(np.asarray(inp["w_dn"])[0].reshape(32, 16, 128, 8, 256).transpose(0, 3, 2, 1, 4))
    sh["bdn"] = f(np.asarray(inp["b_dn"])[0])
    return sh


def _fm(X):
    T = X.shape[0]
    return np.ascontiguousarray(X.T.reshape(16, 128, T).transpose(1, 0, 2), dtype=np.float32)


_NC_CACHE = {}


def kernel(**inp):
    xpr = np.asarray(inp["x_prompt"], dtype=np.float32)
    xsm = np.asarray(inp["x_sample"], dtype=np.float32)
    cpr = np.asarray(inp["c_prompt"], dtype=np.float32)
    csm = np.asarray(inp["c_sample"], dtype=np.float32)
    ckk = np.asarray(inp["cache_k"], dtype=np.float32)[0]
    cvv = np.asarray(inp["cache_v"], dtype=np.float32)[0]
    ccv = np.asarray(inp["cache_conv"], dtype=np.float32)[0]
    sh = _prep_shared(inp)
    in_maps = []
    for c in range(8):
        b, h = c // 2, c % 2
        X = np.concatenate([xpr[b, h * 2048:(h + 1) * 2048], xsm[4 * c:4 * c + 4].reshape(256, 2048)], axis=0)
        m = dict(sh)
        m["xo"] = _fm(X)
        m["xp"] = _fm(xpr[b, 0:2048]) if h == 1 else np.zeros((128, 16, 2048), np.float32)
        m["flag"] = np.full((128, 1), float(h), np.float32)
        C = np.zeros((8, 2048), np.float32)
        C[0] = cpr[b]; C[1:5] = csm[4 * c:4 * c + 4]
        m["cT"] = np.ascontiguousarray(C.T.reshape(16, 128, 8).transpose(1, 0, 2))
        m["ck"] = np.ascontiguousarray(ckk[4 * c:4 * c + 4].transpose(0, 2, 3, 1))
        m["cv"] = np.ascontiguousarray(cvv[4 * c:4 * c + 4].reshape(4, 16, 128, 8, 128).transpose(0, 3, 2, 1, 4))
        m["cconv"] = np.ascontiguousarray(ccv[4 * c:4 * c + 4].reshape(4, 2, 8, 128).transpose(3, 2, 0, 1))
        in_maps.append(m)
    if "nc" not in _NC_CACHE:
        _NC_CACHE["nc"] = build_nc()
    nc = _NC_CACHE["nc"]
    res = run_bass_kernel_spmd(nc, in_maps, core_ids=list(range(8)))
    y_prompt = np.zeros((4, 4096, 2048), np.float32); y_sample = np.zeros((32, 64, 2048), np.float32)
    k_prompt = np.zeros((1, 4, 4096, 8, 128), np.float32); v_prompt = np.zeros((1, 4, 4096, 8, 128), np.float32)
    conv_prompt = np.zeros((1, 4, 2, 1024), np.float32)
    k_sample = np.zeros((1, 32, 64, 8, 128), np.float32); v_sample = np.zeros((1, 32, 64, 8, 128), np.float32)
    conv_sample = np.zeros((1, 32, 2, 1024), np.float32)
    for c in range(8):
        b, h = c // 2, c % 2
        r = res.results[c]
        Y = r["yT"].transpose(2, 1, 0).reshape(2304, 2048)
        y_prompt[b, h * 2048:(h + 1) * 2048] = Y[:2048]
        y_sample[4 * c:4 * c + 4] = Y[2048:].reshape(4, 64, 2048)
        K = r["kTo"].transpose(2, 1, 0)
        k_prompt[0, b, h * 2048:(h + 1) * 2048] = K[:2048]
        k_sample[0, 4 * c:4 * c + 4] = K[2048:].reshape(4, 64, 8, 128)
        V = r["vo"].reshape(2304, 8, 128)
        v_prompt[0, b, h * 2048:(h + 1) * 2048] = V[:2048]
        v_sample[0, 4 * c:4 * c + 4] = V[2048:].reshape(4, 64, 8, 128)
        cvo = r["convo"].transpose(2, 3, 1, 0).reshape(5, 2, 1024)
        if h == 1:
            conv_prompt[0, b] = cvo[0]
        conv_sample[0, 4 * c:4 * c + 4] = cvo[1:5]
    return (y_prompt, y_sample, k_prompt, v_prompt, conv_prompt, k_sample, v_sample, conv_sample)
```

```python
import numpy as np
import concourse.bass as bass
import concourse.mybir as mybir
from concourse.bass_utils import run_bass_kernel_spmd
from contextlib import ExitStack

F32 = mybir.dt.float32
BF16 = mybir.dt.bfloat16
I32 = mybir.dt.int32
U32 = mybir.dt.uint32
AF = mybir.ActivationFunctionType
ALU = mybir.AluOpType
AX = mybir.AxisListType


class Buf:
    __slots__ = ("ap", "w", "r", "name")

    def __init__(self, ap, name=""):
        self.ap = ap
        self.w = {}
        self.r = {}
        self.name = name

    def __getitem__(self, k):
        return self.ap[k]


class Eng:
    def __init__(self, fw, name, sem, dsems):
        self.fw = fw
        self.name = name
        self.sem = sem
        self.count = 0
        self.dsems = dsems
        self.rr = 0
        self.waited = {}
        self.prog = []


class FW:
    def __init__(self, nc, es, n_sp=24, n_pool=12, n_act=8):
        self.nc = nc
        self.es = es
        self.semid = {}
        def mk(name):
            s = es.enter_context(nc.semaphore(name))
            self.semid[id(s)] = s
            return s
        self.pe = Eng(self, "tensor", mk("s_pe"), [])
        self.act = Eng(self, "scalar", mk("s_act"), [[mk("d_act%d" % i), 0] for i in range(n_act)])
        self.dve = Eng(self, "vector", mk("s_dve"), [])
        self.pool = Eng(self, "gpsimd", mk("s_pool"), [[mk("d_pool%d" % i), 0] for i in range(n_pool)])
        self.sp = Eng(self, "sync", None, [[mk("d_sp%d" % i), 0] for i in range(n_sp)])
        self.engs = [self.pe, self.act, self.dve, self.pool, self.sp]
        self.n_inst = 0

    def _needs(self, E, reads, writes, skip_self=False):
        need = {}
        for b in reads:
            for s, v in b.w.items():
                if need.get(s, 0) < v:
                    need[s] = v
        for b in writes:
            for s, v in b.w.items():
                if need.get(s, 0) < v:
                    need[s] = v
            for s, v in b.r.items():
                if need.get(s, 0) < v:
                    need[s] = v
        waits = []
        for s, v in need.items():
            if skip_self and E.sem is not None and s is E.sem:
                continue
            if E.waited.get(id(s), 0) < v:
                E.waited[id(s)] = v
                waits.append((s, v))
        return waits

    def _record(self, tk_sem, tk_val, reads, writes):
        for b in reads:
            if b.r.get(tk_sem, 0) < tk_val:
                b.r[tk_sem] = tk_val
        for b in writes:
            b.w = {tk_sem: tk_val}
            b.r = {}

    def op(self, E, fn, reads=(), writes=(), skip_self=False, inc=True):
        waits = self._needs(E, reads, writes, skip_self=skip_self)
        self.n_inst += 1
        if inc:
            E.count += 1
            sem = E.sem
            def emit(e, waits=waits, fn=fn, sem=sem):
                for s, v in waits:
                    e.wait_ge(s, v)
                fn(e).then_inc(sem, 1)
            E.prog.append(emit)
            self._record(E.sem, E.count, reads, writes)
        else:
            def emit(e, waits=waits, fn=fn):
                for s, v in waits:
                    e.wait_ge(s, v)
                fn(e)
            E.prog.append(emit)

    def dma(self, Q, fn, reads=(), writes=()):
        d = Q.dsems[Q.rr]
        Q.rr = (Q.rr + 1) % len(Q.dsems)
        s = d[0]
        waits = []
        if d[1] > 0 and Q.waited.get(id(s), 0) < d[1]:
            Q.waited[id(s)] = d[1]
            waits.append((s, d[1]))
        waits += self._needs(Q, reads, writes)
        d[1] += 16
        val = d[1]
        self.n_inst += 1
        def emit(e, waits=waits, fn=fn, s=s):
            for ss, v in waits:
                e.wait_ge(ss, v)
            fn(e).then_inc(s, 16)
        Q.prog.append(emit)
        self._record(s, val, reads, writes)

    def finish(self):
        for Q in (self.sp, self.pool, self.act):
            for d in Q.dsems:
                if d[1] > 0:
                    for E in (self.sp,):
                        if E.waited.get(id(d[0]), 0) < d[1]:
                            E.waited[id(d[0])] = d[1]
                            E.prog.append(lambda e, s=d[0], v=d[1]: e.wait_ge(s, v))

    def barrier(self):
        tickets = []
        for E in (self.pe, self.act, self.dve, self.pool):
            if E.count > 0:
                tickets.append((E.sem, E.count))
        for Q in (self.sp, self.pool, self.act):
            for d in Q.dsems:
                if d[1] > 0:
                    tickets.append((d[0], d[1]))
        for E in self.engs:
            for s, v in tickets:
                if s is E.sem:
                    continue
                if E.waited.get(id(s), 0) < v:
                    E.waited[id(s)] = v
                    E.prog.append(lambda e, s=s, v=v: e.wait_ge(s, v))

    def emit_all(self):
        nc = self.nc
        progs = {E.name: E.prog for E in self.engs}
        for E in self.engs:
            E.prog = []
        self._emit(progs)

    def _emit(self, progs):
        nc = self.nc
        with nc.Block() as block:
            @block.tensor
            def _(e):
                for f in progs['tensor']:
                    f(e)
            @block.scalar
            def _(e):
                for f in progs['scalar']:
                    f(e)
            @block.vector
            def _(e):
                for f in progs['vector']:
                    f(e)
            @block.gpsimd
            def _(e):
                for f in progs['gpsimd']:
                    f(e)
            @block.sync
            def _(e):
                for f in progs['sync']:
                    f(e)

import math
D = 2048
KC = 16
NOWN = 2304
NPR = 2048
CAP = 512
NE = 32
ALPHA = 2.0 ** 0.25
QSCALE = 1.0 / math.sqrt(128.0)
EPS = 1e-5
PL = 2 + 2048 + 4 * 66
STOP_AFTER = 99

OWN_GROUPS = [(0, 512), (512, 512), (1024, 512), (1536, 512), (2048, 256)]


def segs_of(t0, n):
    if t0 < 2048:
        return [(0, n, 0)]
    return [(64 * s, 64, 1 + s) for s in range(4)]


def build_nc():
    nc = bass.Bass("TRN2", target_bir_lowering=False)

    def din(name, shape, dt=F32):
        return nc.dram_tensor(name, shape, dt, kind="ExternalInput").ap()

    def dout(name, shape, dt=F32):
        return nc.dram_tensor(name, shape, dt, kind="ExternalOutput").ap()

    def dint(name, shape, dt=F32):
        return nc.dram_tensor(name, shape, dt, kind="Internal").ap()

    xo = din("xo", [128, 16, NOWN]); xp = din("xp", [128, 16, NPR]); flag_d = din("flag", [128, 1])
    cT_d = din("cT", [128, 16, 8]); lnp_d = din("lnp", [128, 6, 16]); bada_d = din("bada", [128, 6, 16])
    wada = din("wada", [24, 128, 16, 512]); win = din("win", [80, 128, 16, 128])
    convw_d = din("convw", [128, 8, 3])
    wbc = din("wbc", [8, 128, 8, 256]); wba = din("wba", [8, 128, 8, 256]); wout = din("wout", [8, 128, 16, 256])
    wr_d = din("wr", [128, 16, 32]); br_d = din("br", [128, 32])
    NEW = 32 if STOP_AFTER >= 5 else 1
    wgu = din("wgu", [NEW, 16, 128, 16, 256]); bgu_d = din("bgu", [128, 32, 32])
    wdn = din("wdn", [NEW, 8, 128, 16, 256]); bdn_d = din("bdn", [32, 2048])
    ck = din("ck", [4, 8, 128, 2048]); cv = din("cv", [4, 8, 128, 16, 128]); cconv = din("cconv", [128, 8, 4, 2])

    yT = dout("yT", [128, 16, NOWN]); kTo = dout("kTo", [128, 8, NOWN]); vo = dout("vo", [NOWN, 1024])
    convo = dout("convo", [128, 8, 5, 2])

    uT_s = dint("uT_s", [128, 16, NOWN]); qT_s = dint("qT_s", [128, 8, NOWN], BF16)
    kT_s = dint("kT_s", [128, 8, NPR + NOWN], BF16); v_s = dint("v_s", [NPR + NOWN, 1024], BF16)
    ycT_s = dint("ycT_s", [128, 8, NOWN], BF16)
    sga_s = dint("sga_s", [128, 16, NOWN], BF16); sgb_s = dint("sgb_s", [128, 16, NOWN], BF16)
    oT_s = dint("oT_s", [128, 8, NOWN], BF16); u1T_s = dint("u1T_s", [128, 16, NOWN])
    xbuf = dint("xbuf", [NE * CAP, 2048], BF16); ybuf = dint("ybuf", [NE * CAP, 2048])

    dbufs = {}

    def db(*key):
        if key not in dbufs:
            dbufs[key] = Buf(None, str(key))
        return dbufs[key]

    with ExitStack() as es:
        fw = FW(nc, es)
        sp, act, dve, pool, pe = fw.sp, fw.act, fw.dve, fw.pool, fw.pe
        cnt = [0]

        def sbt(stack, name, shape, dt):
            cnt[0] += 1
            return Buf(stack.enter_context(nc.sbuf_tensor("%s_%d" % (name, cnt[0]), shape, dt)), name)

        banks = [Buf(es.enter_context(nc.psum_tensor("ps%d" % i, [128, 512], F32)), "ps%d" % i) for i in range(8)]
        bi = [0]

        def pbank():
            b = banks[bi[0] % 8]
            bi[0] += 1
            return b

        def ld(out_ap, in_ap, reads, writes, q=None):
            fw.dma(q or sp, lambda e: e.dma_start(out=out_ap, in_=in_ap), reads=reads, writes=writes)

        def mm_group(pb, out_ap, pairs, reads):
            n = len(pairs)
            for i, (l, r) in enumerate(pairs):
                fw.op(pe, lambda e, l=l, r=r, i=i: e.matmul(out_ap, lhsT=l, rhs=r, start=(i == 0), stop=(i == n - 1)),
                      reads=reads, writes=[pb], skip_self=True, inc=(i == n - 1))

        def tr_group(pb, items, reads):
            n = len(items)
            for i, (o, a, idn) in enumerate(items):
                fw.op(pe, lambda e, o=o, a=a, idn=idn: e.transpose(out=o, in_=a, identity=idn),
                      reads=reads, writes=[pb], skip_self=True, inc=(i == n - 1))

        ident = sbt(es, "ident", [128, 128], F32); identb = sbt(es, "identb", [128, 128], BF16)
        onesD = sbt(es, "onesD", [128, 128], F32); ones1 = sbt(es, "ones1", [128, 128], F32)
        ustr = sbt(es, "ustr", [128, 128], F32)
        maskL = sbt(es, "maskL", [128, 128], F32); maskLb = sbt(es, "maskLb", [128, 128], BF16)
        ones512 = sbt(es, "ones512", [128, 512], F32)
        iota32 = sbt(es, "iota32", [128, 32], F32)
        flag = sbt(es, "flag", [128, 1], F32)
        fw.op(pool, lambda e: e.memset(ident[:], 1.0), writes=[ident])
        fw.op(pool, lambda e: e.affine_select(out=ident[:], in_=ident[:], pattern=[[-1, 128]], compare_op=ALU.is_equal, fill=0.0, base=0, channel_multiplier=1), writes=[ident])
        fw.op(pool, lambda e: e.tensor_copy(out=identb[:], in_=ident[:]), reads=[ident], writes=[identb])
        fw.op(pool, lambda e: e.memset(onesD[:], 1.0 / D), writes=[onesD])
        fw.op(pool, lambda e: e.memset(ones1[:], 1.0), writes=[ones1])
        fw.op(pool, lambda e: e.memset(ones512[:], 1.0), writes=[ones512])
        fw.op(pool, lambda e: e.memset(maskL[:], 1.0), writes=[maskL])
        fw.op(pool, lambda e: e.affine_select(out=maskL[:], in_=maskL[:], pattern=[[-1, 128]], compare_op=ALU.is_gt, fill=0.0, base=0, channel_multiplier=1), writes=[maskL])
        fw.op(pool, lambda e: e.tensor_copy(out=maskLb[:], in_=maskL[:]), reads=[maskL], writes=[maskLb])
        fw.op(pool, lambda e: e.memset(ustr[:], 1.0), writes=[ustr])
        fw.op(pool, lambda e: e.affine_select(out=ustr[:], in_=ustr[:], pattern=[[1, 128]], compare_op=ALU.is_gt, fill=0.0, base=0, channel_multiplier=-1), writes=[ustr])
        fw.op(pool, lambda e: e.iota(iota32[:], pattern=[[1, 32]], base=0, channel_multiplier=0, allow_small_or_imprecise_dtypes=True), writes=[iota32])
        ld(flag[:], flag_d[:, :], [], [flag])

        cT = sbt(es, "cT", [128, 16, 8], F32); lnp = sbt(es, "lnp", [128, 6, 16], F32); bada = sbt(es, "bada", [128, 6, 16], F32)
        modT = sbt(es, "modT", [128, 6, 16, 8], F32)
        A0 = sbt(es, "A0", [128, 16, 8], F32); B0 = sbt(es, "B0", [128, 16, 8], F32)
        A2 = sbt(es, "A2", [128, 16, 8], F32); B2 = sbt(es, "B2", [128, 16, 8], F32)
        agb = sbt(es, "agb", [128, 4, 16], F32)
        convw = sbt(es, "convw", [128, 8, 3], F32)
        wr = sbt(es, "wr", [128, 16, 32], F32); brt = sbt(es, "brt", [128, 32], F32)
        rows_i = sbt(es, "rows_i", [128, 18, 4], I32); gates = sbt(es, "gates", [128, 18, 4], F32)
        combT = sbt(es, "combT", [32, NOWN], F32)
        cntb = sbt(es, "cntb", [128, 32], F32)
        hlast = sbt(es, "hlast", [128, 16, 2], BF16)
        for t, d_ in ((cT, cT_d), (lnp, lnp_d), (bada, bada_d), (convw, convw_d), (wr, wr_d)):
            ld(t[:], d_[:, :, :], [], [t])
        ld(brt[:], br_d[:, :], [], [brt])
        fw.op(pool, lambda e: e.memset(cntb[:], 0.0), writes=[cntb])

        def phase_end():
            fw.barrier()
            fw.emit_all()

        with ExitStack() as ph:
            wst = [sbt(ph, "wada_st", [128, 16, 512], F32) for _ in range(3)]
            modrow = sbt(ph, "modrow", [8, 12288], F32)
            fw.op(dve, lambda e: e.tensor_scalar_add(out=bada[:, 1:3, :], in0=bada[:, 1:3, :], scalar1=1.0), reads=[bada], writes=[bada])
            fw.op(dve, lambda e: e.tensor_scalar_add(out=bada[:, 4:6, :], in0=bada[:, 4:6, :], scalar1=1.0), reads=[bada], writes=[bada])
            for j in range(min(2, 24)):
                ld(wst[j % 3][:], wada[j, :, :, :], [], [wst[j % 3]])
            for j in range(24):
                if j + 2 < 24:
                    ld(wst[(j + 2) % 3][:], wada[j + 2, :, :, :], [], [wst[(j + 2) % 3]])
                w = wst[j % 3]
                pb = pbank()
                mm_group(pb, pb[0:8, :], [(cT[:, kc, :], w[:, kc, :]) for kc in range(16)], [w, cT])
                if j % 2 == 0:
                    fw.op(act, lambda e, pb=pb, j=j: e.copy(out=modrow[0:8, j * 512:(j + 1) * 512], in_=pb[0:8, :]), reads=[pb], writes=[modrow])
                else:
                    fw.op(dve, lambda e, pb=pb, j=j: e.tensor_copy(out=modrow[0:8, j * 512:(j + 1) * 512], in_=pb[0:8, :]), reads=[pb], writes=[modrow])
            for j4 in range(24):
                pb = pbank()
                tr_group(pb, [(pb[:, q * 8:(q + 1) * 8], modrow[0:8, (j4 * 4 + q) * 128:(j4 * 4 + q + 1) * 128], ident[0:8, 0:8]) for q in range(4)], [modrow, ident])
                for q in range(4):
                    jj = j4 * 4 + q
                    v, ch = jj // 16, jj % 16
                    fw.op(act, lambda e, pb=pb, v=v, ch=ch, q=q: e.activation(out=modT[:, v, ch, :], in_=pb[:, q * 8:(q + 1) * 8], func=AF.Identity, bias=bada[:, v, ch:ch + 1], scale=1.0),
                          reads=[pb, bada], writes=[modT])
            def bc3(ap2):
                return ap2.unsqueeze(2).to_broadcast([128, 16, 8])
            fw.op(dve, lambda e: e.tensor_tensor(out=A0[:], in0=modT[:, 1, :, :], in1=bc3(lnp[:, 0, :]), op=ALU.mult), reads=[modT, lnp], writes=[A0])
            fw.op(dve, lambda e: e.tensor_tensor(out=B0[:], in0=modT[:, 1, :, :], in1=bc3(lnp[:, 1, :]), op=ALU.mult), reads=[modT, lnp], writes=[B0])
            fw.op(dve, lambda e: e.tensor_tensor(out=B0[:], in0=B0[:], in1=modT[:, 0, :, :], op=ALU.add), reads=[modT], writes=[B0])
            fw.op(dve, lambda e: e.tensor_tensor(out=A2[:], in0=modT[:, 4, :, :], in1=bc3(lnp[:, 2, :]), op=ALU.mult), reads=[modT, lnp], writes=[A2])
            fw.op(dve, lambda e: e.tensor_tensor(out=B2[:], in0=modT[:, 4, :, :], in1=bc3(lnp[:, 3, :]), op=ALU.mult), reads=[modT, lnp], writes=[B2])
            fw.op(dve, lambda e: e.tensor_tensor(out=B2[:], in0=B2[:], in1=modT[:, 3, :, :], op=ALU.add), reads=[modT], writes=[B2])
            fw.op(dve, lambda e: e.tensor_scalar_mul(out=agb[:], in0=lnp[:, 0:4, :], scalar1=ALPHA), reads=[lnp], writes=[agb])
            phase_end()

        def ln_core(ph_tiles, src, n):
            sq, mean, msq, var, rstd = ph_tiles
            fw.op(act, lambda e: e.activation(out=sq[:, :, 0:n], in_=src[:, :, 0:n], func=AF.Square), reads=[src], writes=[sq])
            p1 = pbank(); p2 = pbank()
            mm_group(p1, p1[:, 0:n], [(onesD[:], src[:, kc, 0:n]) for kc in range(16)], [onesD, src])
            mm_group(p2, p2[:, 0:n], [(onesD[:], sq[:, kc, 0:n]) for kc in range(16)], [onesD, sq])
            fw.op(act, lambda e: e.copy(out=mean[:, 0:n], in_=p1[:, 0:n]), reads=[p1], writes=[mean])
            fw.op(dve, lambda e: e.tensor_tensor(out=msq[:, 0:n], in0=mean[:, 0:n], in1=mean[:, 0:n], op=ALU.mult), reads=[mean], writes=[msq])
            fw.op(dve, lambda e: e.tensor_tensor(out=var[:, 0:n], in0=p2[:, 0:n], in1=msq[:, 0:n], op=ALU.subtract), reads=[p2, msq], writes=[var])
            fw.op(dve, lambda e: e.tensor_scalar_add(out=var[:, 0:n], in0=var[:, 0:n], scalar1=EPS), reads=[var], writes=[var])
            fw.op(act, lambda e: e.activation(out=msq[:, 0:n], in_=var[:, 0:n], func=AF.Sqrt), reads=[var], writes=[msq])
            fw.op(dve, lambda e: e.reciprocal(out=rstd[:, 0:n], in_=msq[:, 0:n]), reads=[msq], writes=[rstd])
            fw.op(dve, lambda e: e.tensor_tensor(out=src[:, :, 0:n], in0=src[:, :, 0:n], in1=mean[:, 0:n].unsqueeze(1).to_broadcast([128, 16, n]), op=ALU.subtract), reads=[mean, sq], writes=[src])
            fw.op(dve, lambda e: e.tensor_tensor(out=src[:, :, 0:n], in0=src[:, :, 0:n], in1=rstd[:, 0:n].unsqueeze(1).to_broadcast([128, 16, n]), op=ALU.mult), reads=[rstd], writes=[src])

        def ln_tiles(ph):
            return (sbt(ph, "sq", [128, 16, 512], F32), sbt(ph, "mean", [128, 512], F32), sbt(ph, "msq", [128, 512], F32),
                    sbt(ph, "var", [128, 512], F32), sbt(ph, "rstd", [128, 512], F32))

        def affine(out_buf, out_fn, src, kc_scale_fn, kc_bias_fn, t0, n, extra_reads):
            for kc in range(16):
                for (o, l, s) in segs_of(t0, n):
                    fw.op(act, lambda e, kc=kc, o=o, l=l, s=s: e.activation(out=out_fn(kc, o, l), in_=src[:, kc, o:o + l], func=AF.Identity,
                                                                        bias=kc_bias_fn(kc, s), scale=kc_scale_fn(kc, s)),
                          reads=[src] + extra_reads, writes=[out_buf])

        class WS:
            def __init__(self, ph, kcmax, bw, nst=3, nwb=3, cast=None):
                self.cast = cast or (pool, dve)
                self.st = [sbt(ph, "wst", [128, kcmax, bw], F32) for _ in range(nst)]
                self.wlo = [sbt(ph, "wlo", [128, kcmax // 2, bw], BF16) for _ in range(nwb)]
                self.whi = [sbt(ph, "whi", [128, kcmax // 2, bw], BF16) for _ in range(nwb)]
                self.i = 0
                self.pending = []

            def issue(self, dram_ap, kc):
                st = self.st[self.i % len(self.st)]
                ld(st[:, 0:kc, :], dram_ap, [], [st])
                self.pending.append((st, kc, self.i))
                self.i += 1

            def get(self):
                st, kc, i = self.pending.pop(0)
                lo = self.wlo[i % len(self.wlo)]; hi = self.whi[i % len(self.whi)]
                h = kc // 2
                c0_, c1_ = self.cast
                if c0_ is act:
                    fw.op(act, lambda e: e.copy(out=lo[:, 0:h, :], in_=st[:, 0:h, :]), reads=[st], writes=[lo])
                else:
                    fw.op(c0_, lambda e: e.tensor_copy(out=lo[:, 0:h, :], in_=st[:, 0:h, :]), reads=[st], writes=[lo])
                fw.op(c1_, lambda e: e.tensor_copy(out=hi[:, 0:h, :], in_=st[:, h:kc, :]), reads=[st], writes=[hi])
                def w(k, a, b):
                    return lo[:, k, a:b] if k < h else hi[:, k - h, a:b]
                return w, [lo, hi]

        def stream(ws, blocks, body, depth=2):
            n = len(blocks)
            for i in range(min(depth + 1, n)):
                ws.issue(blocks[i][0], blocks[i][1])
            cur = ws.get() if n else None
            for i in range(n):
                if i + depth + 1 < n:
                    ws.issue(blocks[i + depth + 1][0], blocks[i + depth + 1][1])
                nxt = ws.get() if i + 1 < n else None
                body(cur[0], cur[1], blocks[i][2])
                cur = nxt

        with ExitStack() as ph12:
            hT = sbt(ph12, "hT", [128, 16, NOWN], BF16)

            def ln0_phase(xsrc, groups, store_u, tok_base):
                with ExitStack() as ph:
                    xg = [sbt(ph, "xg", [128, 16, 512], F32) for _ in range(1)]
                    lt = ln_tiles(ph)
                    sq = lt[0]
                    for gi, (t0, n) in enumerate(groups):
                        x = xg[0]
                        ld(x[:, :, 0:n], xsrc[:, :, t0:t0 + n], [], [x])
                        ln_core(lt, x, n)
                        affine(hT, lambda kc, o, l, t0=t0: hT[:, kc, t0 + o:t0 + o + l], x,
                               lambda kc, s: A0[:, kc, s:s + 1], lambda kc, s: B0[:, kc, s:s + 1], t0 + tok_base, n, [A0, B0])
                        if store_u:
                            affine(sq, lambda kc, o, l: sq[:, kc, o:o + l], x,
                                   lambda kc, s: agb[:, 0, kc:kc + 1], lambda kc, s: agb[:, 1, kc:kc + 1], t0 + tok_base, n, [agb])
                            ld(uT_s[:, :, t0:t0 + n], sq[:, :, 0:n], [sq], [db("uT", gi)])
                    phase_end()

            if STOP_AFTER >= 0:
                ln0_phase(xp, [(0, 512), (512, 512), (1024, 512), (1536, 512)], False, 0)
                fw.op(pool, lambda e: e.tensor_copy(out=hlast[:], in_=hT[:, :, 2046:2048]), reads=[hT], writes=[hlast])

            def proj_phase(ntok, groups, prev):
                with ExitStack() as ph:
                    ws = WS(ph, 16, 128)
                    ev32 = [sbt(ph, "ev32", [128, 512], F32) for _ in range(3)]
                    evb = [sbt(ph, "evb", [128, 512], BF16) for _ in range(3)]
                    ei = [0]
                    kbase = 0 if prev else NPR
                    if not prev:
                        xin = sbt(ph, "xin", [128, PL], F32); acc = sbt(ph, "acc", [128, PL], F32); ycb = sbt(ph, "ycb", [128, PL], BF16)
                        tmpc = sbt(ph, "tmpc", [128, 2], F32)

                    def pcol(t0):
                        return t0 + 2

                    def fm_matmul(w, wb, c0, t0, n):
                        pb = pbank()
                        mm_group(pb, pb[:, 0:n], [(w(kc, c0, c0 + 128), hT[:, kc, t0:t0 + n]) for kc in range(16)], wb + [hT])
                        return pb

                    def body(w, wb, info):
                        kind, idx = info
                        if kind in ("q", "k"):
                            for gi, (t0, n) in enumerate(groups):
                                pb = fm_matmul(w, wb, 0, t0, n)
                                b16 = evb[ei[0] % 3]; f32 = ev32[ei[0] % 3]; ei[0] += 1
                                if kind == "q":
                                    fw.op(act, lambda e, pb=pb, b16=b16, n=n: e.copy(out=b16[:, 0:n], in_=pb[:, 0:n]), reads=[pb], writes=[b16])
                                    ld(qT_s[:, idx, t0:t0 + n], b16[:, 0:n], [b16], [db("qT", idx, gi)])
                                else:
                                    fw.op(act, lambda e, pb=pb, f32=f32, n=n: e.copy(out=f32[:, 0:n], in_=pb[:, 0:n]), reads=[pb], writes=[f32])
                                    fw.op(dve, lambda e, f32=f32, b16=b16, n=n: e.tensor_copy(out=b16[:, 0:n], in_=f32[:, 0:n]), reads=[f32], writes=[b16])
                                    ld(kT_s[:, idx, kbase + t0:kbase + t0 + n], b16[:, 0:n], [b16], [db("kT", idx, prev, gi)])
                                    if not prev:
                                        ld(kTo[:, idx, t0:t0 + n], f32[:, 0:n], [f32], [db("kTo", idx, gi)])
                        elif kind == "v":
                            for tt in range(ntok // 128):
                                pb = pbank()
                                mm_group(pb, pb[:, 0:128], [(hT[:, kc, tt * 128:(tt + 1) * 128], w(kc, 0, 128)) for kc in range(16)], wb + [hT])
                                b16 = evb[ei[0] % 3]; f32 = ev32[ei[0] % 3]; ei[0] += 1
                                r0 = kbase + tt * 128
                                if prev:
                                    fw.op(dve, lambda e, pb=pb, b16=b16: e.tensor_scalar(out=b16[:, 0:128], in0=pb[:, 0:128], scalar1=flag[:, 0:1], scalar2=None, op0=ALU.mult), reads=[pb, flag], writes=[b16])
                                else:
                                    fw.op(act, lambda e, pb=pb, f32=f32: e.copy(out=f32[:, 0:128], in_=pb[:, 0:128]), reads=[pb], writes=[f32])
                                    fw.op(dve, lambda e, f32=f32, b16=b16: e.tensor_copy(out=b16[:, 0:128], in_=f32[:, 0:128]), reads=[f32], writes=[b16])
                                    ld(vo[tt * 128:(tt + 1) * 128, idx * 128:(idx + 1) * 128], f32[:, 0:128], [f32], [db("vo", idx, tt)])
                                ld(v_s[r0:r0 + 128, idx * 128:(idx + 1) * 128], b16[:, 0:128], [b16], [db("v_s", idx, prev, tt)])
                        elif kind in ("ga", "gb"):
                            dst = sga_s if kind == "ga" else sgb_s
                            for gi, (t0, n) in enumerate(groups):
                                pb = fm_matmul(w, wb, 0, t0, n)
                                b16 = evb[ei[0] % 3]; ei[0] += 1
                                fw.op(act, lambda e, pb=pb, b16=b16, n=n: e.activation(out=b16[:, 0:n], in_=pb[:, 0:n], func=AF.Sigmoid), reads=[pb], writes=[b16])
                                ld(dst[:, idx, t0:t0 + n], b16[:, 0:n], [b16], [db(kind, idx, gi)])
                        elif kind in ("xc", "cc", "bc"):
                            if kind == "xc":
                                ld(xin[:, 2050:2050 + 264].rearrange("p (s c) -> p s c", c=66)[:, :, 0:2], cconv[:, idx, :, :], [], [xin])
                                pbh = pbank()
                                mm_group(pbh, pbh[:, 0:2], [(w(kc, 0, 128), hlast[:, kc, :]) for kc in range(16)], wb + [hlast])
                                fw.op(act, lambda e, pbh=pbh: e.copy(out=xin[:, 0:2], in_=pbh[:, 0:2]), reads=[pbh], writes=[xin])
                            if kind == "cc":
                                pbh = pbank()
                                mm_group(pbh, pbh[:, 0:2], [(w(kc, 0, 128), hlast[:, kc, :]) for kc in range(16)], wb + [hlast])
                                fw.op(dve, lambda e, pbh=pbh: e.scalar_tensor_tensor(out=xin[:, 0:2], in0=pbh[:, 0:2], scalar=flag[:, 0:1], in1=xin[:, 0:2], op0=ALU.mult, op1=ALU.mult), reads=[pbh, flag], writes=[xin])
                            for gi, (t0, n) in enumerate(groups):
                                pb = fm_matmul(w, wb, 0, t0, n)
                                for (o, l, s) in segs_of(t0, n):
                                    c0 = (t0 + 2 + o) if s == 0 else (2052 + 66 * (s - 1))
                                    if kind == "xc":
                                        fw.op(act, lambda e, pb=pb, o=o, l=l, c0=c0: e.copy(out=xin[:, c0:c0 + l], in_=pb[:, o:o + l]), reads=[pb], writes=[xin])
                                    elif kind == "cc":
                                        fw.op(dve, lambda e, pb=pb, o=o, l=l, c0=c0: e.tensor_tensor(out=xin[:, c0:c0 + l], in0=pb[:, o:o + l], in1=xin[:, c0:c0 + l], op=ALU.mult), reads=[pb], writes=[xin])
                                    else:
                                        fw.op(dve, lambda e, pb=pb, o=o, l=l, c0=c0: e.tensor_tensor(out=ycb[:, c0:c0 + l], in0=pb[:, o:o + l], in1=acc[:, c0:c0 + l], op=ALU.mult), reads=[pb, acc], writes=[ycb])
                            if kind == "cc":
                                ld(convo[:, idx, 0, :], xin[:, 2048:2050], [xin], [db("convo", idx, 0)])
                                ld(convo[:, idx, 1:5, :], xin[:, 2050:2050 + 264].rearrange("p (s c) -> p s c", c=66)[:, :, 64:66], [xin], [db("convo", idx, 1)])
                                fw.op(pool, lambda e: e.tensor_scalar(out=acc[:, 2:PL], in0=xin[:, 0:PL - 2], scalar1=convw[:, idx, 0:1], scalar2=None, op0=ALU.mult), reads=[xin, convw], writes=[acc])
                                fw.op(dve, lambda e: e.scalar_tensor_tensor(out=acc[:, 2:PL], in0=xin[:, 1:PL - 1], scalar=convw[:, idx, 1:2], in1=acc[:, 2:PL], op0=ALU.mult, op1=ALU.add), reads=[xin, convw], writes=[acc])
                                fw.op(dve, lambda e: e.scalar_tensor_tensor(out=acc[:, 2:PL], in0=xin[:, 2:PL], scalar=convw[:, idx, 2:3], in1=acc[:, 2:PL], op0=ALU.mult, op1=ALU.add), reads=[xin, convw], writes=[acc])
                            if kind == "bc":
                                ld(ycT_s[:, idx, 0:2048], ycb[:, 2:2050], [ycb], [db("ycT", idx, 0)])
                                ld(ycT_s[:, idx, 2048:2304].rearrange("p (s c) -> p s c", c=64), ycb[:, 2050:2050 + 264].rearrange("p (s c) -> p s c", c=66)[:, :, 2:66], [ycb], [db("ycT", idx, 1)])

                    blocks = []
                    if prev:
                        import os
                        if os.environ.get("DBG", "kv").find("k") >= 0:
                          for i in range(8):
                            blocks.append((win[32 + i, :, :, :], 16, ("k", i)))
                        if os.environ.get("DBG", "kv").find("v") >= 0:
                          for i in range(8):
                            blocks.append((win[40 + i, :, :, :], 16, ("v", i)))
                    else:
                        for i in range(8):
                            blocks.append((win[i, :, :, :], 16, ("xc", i)))
                            blocks.append((win[16 + i, :, :, :], 16, ("cc", i)))
                            blocks.append((win[8 + i, :, :, :], 16, ("bc", i)))
                        for i in range(8):
                            blocks.append((win[24 + i, :, :, :], 16, ("q", i)))
                        for i in range(8):
                            blocks.append((win[32 + i, :, :, :], 16, ("k", i)))
                        for i in range(8):
                            blocks.append((win[40 + i, :, :, :], 16, ("v", i)))
                        for i in range(16):
                            blocks.append((win[48 + i, :, :, :], 16, ("ga", i)))
                        for i in range(16):
                            blocks.append((win[64 + i, :, :, :], 16, ("gb", i)))
                    stream(ws, blocks, body)
                    phase_end()

            if STOP_AFTER >= 1:
                proj_phase(NPR, [(0, 512), (512, 512), (1024, 512), (1536, 512)], True)
            if STOP_AFTER >= 2:
                ln0_phase(xo, OWN_GROUPS, True, 0)
                proj_phase(NOWN, OWN_GROUPS, False)

        if STOP_AFTER >= 3:
          with ExitStack() as ph:
            qh = [sbt(ph, "qh", [128, 2048], BF16) for _ in range(2)]
            kh = [sbt(ph, "kh", [128, 4096], BF16) for _ in range(2)]
            vh = [sbt(ph, "vh", [128, 32, 128], BF16) for _ in range(2)]
            oh = [sbt(ph, "oh", [128, 2048], BF16) for _ in range(2)]
            kc32 = [sbt(ph, "kc32", [128, 2048], F32) for _ in range(2)]
            vc32 = [sbt(ph, "vc32", [128, 16, 128], F32) for _ in range(2)]
            kcb = [sbt(ph, "kcb", [128, 2048], BF16) for _ in range(2)]
            vcb = [sbt(ph, "vcb", [128, 16, 128], BF16) for _ in range(2)]
            qs = [sbt(ph, "qs", [128, 64], BF16) for _ in range(2)]
            kn = [sbt(ph, "kn", [128, 64], BF16) for _ in range(2)]
            vn = [sbt(ph, "vn", [64, 128], BF16) for _ in range(2)]
            osm = [sbt(ph, "osm", [128, 64], BF16) for _ in range(2)]
            NR = 7
            eb = [sbt(ph, "eb", [128, 512], F32) for _ in range(NR)]
            spb = [sbt(ph, "spb", [128, 512], F32) for _ in range(NR)]
            csb = [sbt(ph, "csb", [128, 512], F32) for _ in range(NR)]
            ddb = [sbt(ph, "ddb", [128, 512], F32) for _ in range(NR)]
            wbb = [sbt(ph, "wbb", [128, 512], BF16) for _ in range(NR)]
            wTb = [sbt(ph, "wTb", [128, 512], BF16) for _ in range(NR)]
            ri = [0]

            ai = [0]
            b6 = [0]
            zi = [0]
            ti3 = [0]

            def pbank():
                b = banks[b6[0] % 6]
                b6[0] += 1
                return b

            jobs = []

            def attend(nq, q_ap, q_reads, chunks, out_fn, pre=None):
                st = {"po": None, "carry": None, "si": 0}
                total = sum(len(c[3]) for c in chunks)
                nch = len(chunks)
                for ci, (kT_ap, C, diag, vts, creads) in enumerate(chunks):
                    jb = {}

                    def A(jb=jb, ci=ci, kT_ap=kT_ap, C=C, diag=diag, creads=creads):
                        if ci == 0:
                            if pre is not None:
                                pre()
                        r = ri[0] % NR; ri[0] += 1
                        jb["r"] = r
                        pz = banks[zi[0] % 4]; zi[0] += 1
                        jb["pz"] = pz
                        mm_group(pz, pz[0:nq, 0:C], [(q_ap, kT_ap)], q_reads + creads)

                    def A1(jb=jb, ci=ci, C=C, diag=diag):
                        r = jb["r"]; pz = jb["pz"]
                        e_, s_ = eb[r], spb[r]
                        fw.op(act, lambda e: e.activation(out=e_[0:nq, 0:C], in_=pz[0:nq, 0:C], func=AF.Exp, scale=QSCALE), reads=[pz], writes=[e_])
                        fw.op(act, lambda e: e.activation(out=s_[0:nq, 0:C], in_=e_[0:nq, 0:C], func=AF.Ln, bias=1.0, scale=1.0), reads=[e_], writes=[s_])
                        if diag:
                            fw.op(pool, lambda e: e.tensor_tensor(out=s_[0:nq, 0:C], in0=s_[0:nq, 0:C], in1=maskL[0:nq, 0:C], op=ALU.mult), reads=[maskL], writes=[s_])

                    def B(jb=jb, ci=ci, kT_ap=kT_ap, C=C, diag=diag, vts=vts, creads=creads):
                        r = jb["r"]; pz = jb["pz"]
                        s_, cs, d_, w_, wT = spb[r], csb[r], ddb[r], wbb[r], wTb[r]
                        if ci == 0:
                            st["po"] = banks[6 + ai[0] % 2]
                            ai[0] += 1
                        po = st["po"]
                        carry = st["carry"]
                        if carry is None:
                            fw.op(dve, lambda e: e.tensor_tensor_scan(out=cs[0:nq, 0:C][:, ::-1], data0=ones512[0:nq, 0:C], data1=s_[0:nq, 0:C][:, ::-1], initial=0.0, op0=ALU.mult, op1=ALU.add),
                                  reads=[s_, ones512], writes=[cs])
                        else:
                            fw.op(dve, lambda e: e.tensor_tensor_scan(out=cs[0:nq, 0:C][:, ::-1], data0=ones512[0:nq, 0:C], data1=s_[0:nq, 0:C][:, ::-1], initial=carry[0:nq, 0:1], op0=ALU.mult, op1=ALU.add),
                                  reads=[s_, ones512, carry], writes=[cs])
                        st["carry"] = cs
                        e_ = eb[r]
                        fw.op(act, lambda e: e.activation(out=d_[0:nq, 0:C], in_=cs[0:nq, 0:C], func=AF.Exp, scale=-1.0), reads=[cs], writes=[d_])

                    def Cst(jb=jb, ci=ci, C=C, diag=diag, vts=vts):
                        r = jb["r"]
                        e_, d_, w_ = eb[r], ddb[r], wbb[r]
                        fw.op(pool, lambda e: e.tensor_tensor(out=w_[0:nq, 0:C], in0=e_[0:nq, 0:C], in1=d_[0:nq, 0:C], op=ALU.mult), reads=[e_, d_], writes=[w_])
                        if diag:
                            fw.op(pool, lambda e: e.tensor_tensor(out=w_[0:nq, 0:C], in0=w_[0:nq, 0:C], in1=maskLb[0:nq, 0:C], op=ALU.mult), reads=[maskLb], writes=[w_])
                        nsub = len(vts)
                        pt = banks[4 + ti3[0] % 2]; ti3[0] += 1
                        jb["pt"] = pt
                        ptb = pt[:].bitcast(BF16)
                        items = []
                        for j, (v_ap, K) in enumerate(vts):
                            items.append((ptb[0:K, j * 128:j * 128 + nq], w_[0:nq, j * 128:j * 128 + K], identb[0:nq, 0:nq]))
                        tr_group(pt, items, [w_, identb])

                    def B2(jb=jb, ci=ci, C=C, vts=vts, creads=creads):
                        r = jb["r"]; pt = jb["pt"]
                        wT = wTb[r]
                        po = st["po"]
                        nsub = len(vts)
                        ptb = pt[:].bitcast(BF16)
                        Kmax = max(K for _, K in vts)
                        src = ptb[0:Kmax, 0:nsub * 128].rearrange("p (j c) -> p j c", c=128)[:, :, 0:nq]
                        dst = wT[0:Kmax, 0:nsub * 128].rearrange("p (j c) -> p j c", c=128)[:, :, 0:nq]
                        ri[0] += 0
                        fw.op(dve, lambda e: e.tensor_copy(out=dst, in_=src), reads=[pt], writes=[wT])
                        for j, (v_ap, K) in enumerate(vts):
                            si = st["si"]
                            fw.op(pe, lambda e, v_ap=v_ap, K=K, j=j, si=si: e.matmul(po[:, 0:nq], lhsT=v_ap, rhs=wT[0:K, j * 128:j * 128 + nq], start=(si == 0), stop=(si == total - 1)),
                                  reads=[wT] + creads, writes=[po], skip_self=True, inc=(j == nsub - 1))
                            st["si"] += 1
                        if ci == nch - 1:
                            out_fn(po)

                    jobs.append((A, A1, B, Cst, B2))

            def run_jobs(la=2):
                n = len(jobs)
                for i in range(-1, n + la + 2):
                    if 0 <= i + 1 < n:
                        jobs[i + 1][0]()
                    if 0 <= i < n:
                        jobs[i][1]()
                    if 0 <= i - la < n:
                        jobs[i - la][2]()
                    if 0 <= i - la - 1 < n:
                        jobs[i - la - 1][3]()
                    if 0 <= i - la - 2 < n:
                        jobs[i - la - 2][4]()
                del jobs[:]

            for h in range(8):
                q_, k_, v_, o_ = qh[h % 2], kh[h % 2], vh[h % 2], oh[h % 2]

                def pre_p(h=h):
                    q2, k2, v2 = qh[h % 2], kh[h % 2], vh[h % 2]
                    ld(q2[:], qT_s[:, h, 0:2048], [db("qT", h, g) for g in range(4)], [q2])
                    ld(k2[:], kT_s[:, h, 0:4096], [db("kT", h, True, g) for g in range(4)] + [db("kT", h, False, g) for g in range(4)], [k2])
                    ld(v2[:], v_s[0:4096, h * 128:(h + 1) * 128].rearrange("(t p) d -> p t d", p=128),
                       [db("v_s", h, True, t) for t in range(16)] + [db("v_s", h, False, t) for t in range(16)], [v2])
                for qi in range(16):
                    chunks = []
                    d0 = NPR + qi * 128
                    chunks.append((k_[:, d0:d0 + 128], 128, True, [(v_[:, 16 + qi, :], 128)], [k_, v_]))
                    L = d0
                    for st in reversed(range(0, L, 512)):
                        C = min(512, L - st)
                        chunks.append((k_[:, st:st + C], C, False, [(v_[:, st // 128 + j, :], 128) for j in range(C // 128)], [k_, v_]))
                    def outp(po, qi=qi, o_=o_, h=h):
                        fw.op(act, lambda e: e.copy(out=o_[:, qi * 128:(qi + 1) * 128], in_=po[:, 0:128]), reads=[po], writes=[o_])
                        if qi == 15:
                            ld(oT_s[:, h, 0:2048], o_[:], [o_], [db("oT", h, 0)])
                    if qi == 0 and h == 0:
                        pre_fn = pre_p
                    elif qi == 2 and h + 1 < 8:
                        pre_fn = (lambda hh=h + 1: pre_p(hh))
                    else:
                        pre_fn = None
                    attend(128, q_[:, qi * 128:(qi + 1) * 128], [q_], chunks, outp, pre=pre_fn)

            ui = 0
            for s in range(4):
                for h in range(8):
                    b = ui % 2; ui += 1
                    t0 = 2048 + 64 * s

                    def pre_s(b=b, s=s, h=h, t0=t0):
                        ld(kc32[b][:], ck[s, h, :, :], [], [kc32[b]])
                        ld(vc32[b][:], cv[s, h, :, :, :], [], [vc32[b]])
                        ld(qs[b][:], qT_s[:, h, t0:t0 + 64], [db("qT", h, 4)], [qs[b]])
                        ld(kn[b][:], kT_s[:, h, NPR + t0:NPR + t0 + 64], [db("kT", h, False, 4)], [kn[b]])
                        ld(vn[b][:], v_s[NPR + t0:NPR + t0 + 64, h * 128:(h + 1) * 128], [db("v_s", h, False, 16 + s // 2)], [vn[b]])
                        fw.op(dve, lambda e: e.tensor_copy(out=kcb[b][:], in_=kc32[b][:]), reads=[kc32[b]], writes=[kcb[b]])
                        fw.op(act, lambda e: e.copy(out=vcb[b][:], in_=vc32[b][:]), reads=[vc32[b]], writes=[vcb[b]])
                    chunks = [(kn[b][:, 0:64], 64, True, [(vn[b][0:64, :], 64)], [kn[b], vn[b]])]
                    for st in (1536, 1024, 512, 0):
                        chunks.append((kcb[b][:, st:st + 512], 512, False, [(vcb[b][:, st // 128 + j, :], 128) for j in range(4)], [kcb[b], vcb[b]]))
                    def outp(po, b=b, h=h, t0=t0):
                        fw.op(act, lambda e: e.copy(out=osm[b][:], in_=po[:, 0:64]), reads=[po], writes=[osm[b]])
                        ld(oT_s[:, h, t0:t0 + 64], osm[b][:], [osm[b]], [db("oT", h, 1 + (t0 - 2048) // 64)])
                    attend(64, qs[b][:], [qs[b]], chunks, outp, pre=pre_s)
            run_jobs(2)
            phase_end()

        if STOP_AFTER >= 4:
          with ExitStack() as ph:
            ws = WS(ph, 16, 256, nst=2, nwb=3)
            ycg = sbt(ph, "ycg", [128, 8, 512], BF16); og = sbt(ph, "og", [128, 8, 512], BF16)
            mT = sbt(ph, "mT", [128, 16, 512], BF16); rT = sbt(ph, "rT", [128, 16, 512], F32)
            lt = ln_tiles(ph); sq = lt[0]
            sga_t = [sbt(ph, "sga_t", [128, 512], BF16) for _ in range(2)]
            sgb_t = [sbt(ph, "sgb_t", [128, 512], BF16) for _ in range(2)]
            au_t = [sbt(ph, "au_t", [128, 512], F32) for _ in range(2)]
            m1 = sbt(ph, "m1", [128, 512], F32); m2 = sbt(ph, "m2", [128, 512], F32)
            h2tok = [sbt(ph, "h2tok", [128, 2048], BF16) for _ in range(1)]
            lg = sbt(ph, "lg", [128, 32], F32); mx8 = sbt(ph, "mx8", [128, 8], F32); idx8 = sbt(ph, "idx8", [128, 8], U32)
            negmax = sbt(ph, "negmax", [128, 1], F32); ek = sbt(ph, "ek", [128, 4], F32); ssum = sbt(ph, "ssum", [128, 1], F32)
            rs = sbt(ph, "rs", [128, 1], F32); Mk = sbt(ph, "Mk", [128, 32], F32); pos = sbt(ph, "pos", [128, 32], F32)
            ohk = sbt(ph, "ohk", [128, 32], F32); tmp = sbt(ph, "tmp", [128, 32], F32); posk = sbt(ph, "posk", [128, 4], F32)
            ekf = sbt(ph, "ekf", [128, 4], F32); rowf = sbt(ph, "rowf", [128, 4], F32); comb = sbt(ph, "comb", [128, 32], F32)
            ti = [0]
            for gi, (t0, n) in enumerate(OWN_GROUPS):
                ld(ycg[:, :, 0:n], ycT_s[:, :, t0:t0 + n], [db("ycT", i, 0 if t0 < 2048 else 1) for i in range(8)], [ycg])
                ld(og[:, :, 0:n], oT_s[:, :, t0:t0 + n], [db("oT", hh, 0) for hh in range(8)] if t0 < 2048 else [db("oT", hh, 1 + s_) for hh in range(8) for s_ in range(4)], [og])
                held = {}

                def body1(w, wb, info, t0=t0, n=n, gi=gi):
                    kind, j = info
                    if kind == "c":
                        held["c"] = (w, wb)
                        return
                    wc, wcb = held["c"]
                    for half in range(2):
                        ft = 2 * j + half
                        r = ti[0] % 2; ti[0] += 1
                        ld(sga_t[r][:, 0:n], sga_s[:, ft, t0:t0 + n], [db("ga", ft, gi)], [sga_t[r]])
                        ld(sgb_t[r][:, 0:n], sgb_s[:, ft, t0:t0 + n], [db("gb", ft, gi)], [sgb_t[r]])
                        pc = pbank(); pa = pbank()
                        mm_group(pc, pc[:, 0:n], [(wc(kc, half * 128, half * 128 + 128), ycg[:, kc, 0:n]) for kc in range(8)], wcb + [ycg])
                        mm_group(pa, pa[:, 0:n], [(w(kc, half * 128, half * 128 + 128), og[:, kc, 0:n]) for kc in range(8)], wb + [og])
                        fw.op(dve, lambda e, pc=pc, r=r: e.tensor_tensor(out=m1[:, 0:n], in0=pc[:, 0:n], in1=sga_t[r][:, 0:n], op=ALU.mult), reads=[pc, sga_t[r]], writes=[m1])
                        fw.op(dve, lambda e, pa=pa, r=r: e.tensor_tensor(out=m2[:, 0:n], in0=pa[:, 0:n], in1=sgb_t[r][:, 0:n], op=ALU.mult), reads=[pa, sgb_t[r]], writes=[m2])
                        fw.op(pool, lambda e, ft=ft: e.tensor_tensor(out=mT[:, ft, 0:n], in0=m1[:, 0:n], in1=m2[:, 0:n], op=ALU.add), reads=[m1, m2], writes=[mT])

                blocks = []
                for j in range(8):
                    blocks.append((wbc[j, :, :, :], 8, ("c", j)))
                    blocks.append((wba[j, :, :, :], 8, ("a", j)))
                stream(ws, blocks, body1, depth=1)

                def body2(w, wb, info, t0=t0, n=n, gi=gi):
                    j = info
                    for half in range(2):
                        ft = 2 * j + half
                        r = ti[0] % 2; ti[0] += 1
                        ld(au_t[r][:, 0:n], uT_s[:, ft, t0:t0 + n], [db("uT", gi)], [au_t[r]])
                        pm = pbank()
                        mm_group(pm, pm[:, 0:n], [(w(kc, half * 128, half * 128 + 128), mT[:, kc, 0:n]) for kc in range(16)], wb + [mT])
                        for (o, l, s) in segs_of(t0, n):
                            fw.op(dve, lambda e, pm=pm, r=r, ft=ft, o=o, l=l, s=s: e.scalar_tensor_tensor(out=rT[:, ft, o:o + l], in0=pm[:, o:o + l], scalar=modT[:, 2, ft, s:s + 1], in1=au_t[r][:, o:o + l], op0=ALU.mult, op1=ALU.add),
                                  reads=[pm, modT, au_t[r]], writes=[rT])

                stream(ws, [(wout[j, :, :, :], 16, j) for j in range(8)], body2, depth=1)
                ln_core(lt, rT, n)
                affine(sq, lambda kc, o, l: sq[:, kc, o:o + l], rT, lambda kc, s: agb[:, 2, kc:kc + 1], lambda kc, s: agb[:, 3, kc:kc + 1], t0, n, [agb])
                ld(u1T_s[:, :, t0:t0 + n], sq[:, :, 0:n], [sq], [db("u1T", gi)])
                affine(rT, lambda kc, o, l: rT[:, kc, o:o + l], rT, lambda kc, s: A2[:, kc, s:s + 1], lambda kc, s: B2[:, kc, s:s + 1], t0, n, [A2, B2])
                for tt in range(n // 128):
                    T = (t0 + tt * 128) // 128
                    c0 = tt * 128
                    pl = pbank()
                    mm_group(pl, pl[:, 0:32], [(rT[:, kc, c0:c0 + 128], wr[:, kc, :]) for kc in range(16)], [rT, wr])
                    fw.op(dve, lambda e, pl=pl: e.tensor_tensor(out=lg[:], in0=pl[:, 0:32], in1=brt[:], op=ALU.add), reads=[pl, brt], writes=[lg])
                    fw.op(dve, lambda e: e.max(out=mx8[:], in_=lg[:]), reads=[lg], writes=[mx8])
                    fw.op(dve, lambda e: e.max_index(out=idx8[:], in_max=mx8[:], in_values=lg[:]), reads=[lg, mx8], writes=[idx8])
                    fw.op(dve, lambda e: e.tensor_scalar_mul(out=negmax[:], in0=mx8[:, 0:1], scalar1=-1.0), reads=[mx8], writes=[negmax])
                    fw.op(act, lambda e: e.activation(out=ek[:], in_=mx8[:, 0:4], func=AF.Exp, bias=negmax[:, 0:1], scale=1.0, accum_out=ssum[:, 0:1]), reads=[mx8, negmax], writes=[ek, ssum])
                    fw.op(dve, lambda e: e.reciprocal(out=rs[:], in_=ssum[:]), reads=[ssum], writes=[rs])
                    fw.op(dve, lambda e, T=T: e.tensor_scalar_mul(out=gates[:, T, :], in0=ek[:], scalar1=rs[:, 0:1]), reads=[ek, rs], writes=[gates])
                    fw.op(dve, lambda e: e.tensor_scalar(out=Mk[:], in0=lg[:], scalar1=mx8[:, 3:4], scalar2=None, op0=ALU.is_ge), reads=[lg, mx8], writes=[Mk])
                    pp = pbank()
                    mm_group(pp, pp[:, 0:32], [(ustr[:], Mk[:])], [ustr, Mk])
                    fw.op(dve, lambda e, pp=pp: e.tensor_tensor(out=pos[:], in0=pp[:, 0:32], in1=cntb[:], op=ALU.add), reads=[pp, cntb], writes=[pos])
                    pc2 = pbank()
                    mm_group(pc2, pc2[:, 0:32], [(ones1[:], Mk[:])], [ones1, Mk])
                    fw.op(dve, lambda e, pc2=pc2: e.tensor_tensor(out=cntb[:], in0=pc2[:, 0:32], in1=cntb[:], op=ALU.add), reads=[pc2], writes=[cntb])
                    fw.op(dve, lambda e: e.tensor_copy(out=ekf[:], in_=idx8[:, 0:4]), reads=[idx8], writes=[ekf])
                    for k in range(4):
                        fw.op(dve, lambda e, k=k: e.tensor_scalar(out=ohk[:], in0=iota32[:], scalar1=ekf[:, k:k + 1], scalar2=None, op0=ALU.is_equal), reads=[iota32, ekf], writes=[ohk])
                        fw.op(dve, lambda e: e.tensor_tensor(out=tmp[:], in0=ohk[:], in1=pos[:], op=ALU.mult), reads=[ohk, pos], writes=[tmp])
                        fw.op(dve, lambda e, k=k: e.reduce_sum(out=posk[:, k:k + 1], in_=tmp[:], axis=AX.X), reads=[tmp], writes=[posk])
                        if k == 0:
                            fw.op(dve, lambda e, T=T: e.tensor_scalar_mul(out=comb[:], in0=ohk[:], scalar1=gates[:, T, 0:1]), reads=[ohk, gates], writes=[comb])
                        else:
                            fw.op(dve, lambda e, T=T, k=k: e.scalar_tensor_tensor(out=comb[:], in0=ohk[:], scalar=gates[:, T, k:k + 1], in1=comb[:], op0=ALU.mult, op1=ALU.add), reads=[ohk, gates], writes=[comb])
                    fw.op(dve, lambda e: e.scalar_tensor_tensor(out=rowf[:], in0=ekf[:], scalar=float(CAP), in1=posk[:], op0=ALU.mult, op1=ALU.add), reads=[ekf, posk], writes=[rowf])
                    fw.op(dve, lambda e, T=T: e.tensor_copy(out=rows_i[:, T, :], in_=rowf[:]), reads=[rowf], writes=[rows_i])
                    ptc = pbank()
                    tr_group(ptc, [(ptc[0:32, 0:128], comb[:], ident[:])], [comb, ident])
                    fw.op(act, lambda e, ptc=ptc, T=T: e.copy(out=combT[:, T * 128:(T + 1) * 128], in_=ptc[0:32, 0:128]), reads=[ptc], writes=[combT])
                    hb = h2tok[0]
                    for q4 in range(4):
                        pt = pbank()
                        tr_group(pt, [(pt[:, j * 128:(j + 1) * 128], rT[:, q4 * 4 + j, c0:c0 + 128], ident[:]) for j in range(4)], [rT, ident])
                        if q4 % 2 == 0:
                            fw.op(act, lambda e, pt=pt, q4=q4, hb=hb: e.copy(out=hb[:, q4 * 512:(q4 + 1) * 512], in_=pt[:, :]), reads=[pt], writes=[hb])
                        else:
                            fw.op(dve, lambda e, pt=pt, q4=q4, hb=hb: e.tensor_copy(out=hb[:, q4 * 512:(q4 + 1) * 512], in_=pt[:, :]), reads=[pt], writes=[hb])
                    for k in range(4):
                        fw.dma(pool, lambda e, T=T, k=k, hb=hb: e.indirect_dma_start(out=xbuf[:, :], out_offset=bass.IndirectOffsetOnAxis(ap=rows_i[:, T, k:k + 1], axis=0), in_=hb[:], in_offset=None),
                               reads=[hb, rows_i], writes=[db("xbuf")])
            phase_end()

        if STOP_AFTER >= 5:
          with ExitStack() as ph:
            ws = WS(ph, 16, 256, nst=3, nwb=2, cast=(act, dve))
            bgu = sbt(ph, "bgu", [128, 32, 32], F32)
            ld(bgu[:], bgu_d[:, :, :], [], [bgu])
            Xe = sbt(ph, "Xe", [128, 4, 2048], BF16)
            XeTs = [sbt(ph, "XeT", [128, 16, 512], BF16) for _ in range(2)]
            gs = sbt(ph, "gs", [128, 16, 512], BF16); actT = sbt(ph, "actT", [128, 16, 512], BF16)
            ytb = [sbt(ph, "ytb", [128, 4, 256], F32) for _ in range(2)]

            def load_xe(e_):
                ld(Xe[:], xbuf[e_ * CAP:(e_ + 1) * CAP, :].rearrange("(t p) d -> p t d", p=128), [db("xbuf")], [Xe])

            def transpose_xe(e_):
                XeT = XeTs[e_ % 2]
                for kc in range(16):
                    pt = pbank(); ptb = pt[:].bitcast(BF16)
                    tr_group(pt, [(ptb[:, tt * 128:(tt + 1) * 128], Xe[:, tt, kc * 128:(kc + 1) * 128], identb[:]) for tt in range(4)], [Xe, identb])
                    if kc % 2 == 0:
                        fw.op(act, lambda e, ptb=ptb, kc=kc: e.copy(out=XeT[:, kc, :], in_=ptb[:, 0:512]), reads=[pt], writes=[XeT])
                    else:
                        fw.op(dve, lambda e, ptb=ptb, kc=kc: e.tensor_copy(out=XeT[:, kc, :], in_=ptb[:, 0:512]), reads=[pt], writes=[XeT])

            load_xe(0)
            transpose_xe(0)
            gtmp = [sbt(ph, "gtmp", [128, 512], F32) for _ in range(2)]
            sgt = [sbt(ph, "sgt", [128, 512], F32) for _ in range(2)]
            ti = [0]

            def body(w, wb, info):
                e_, kind, j = info
                XeT = XeTs[e_ % 2]
                if kind == "u" and j == 0 and e_ + 1 < NE:
                    load_xe(e_ + 1)
                if kind == "d" and j == 0 and e_ + 1 < NE:
                    transpose_xe(e_ + 1)
                if kind in ("g", "u"):
                    for half in range(2):
                        ft = 2 * j + half
                        r = ti[0] % 2; ti[0] += 1
                        pg = pbank()
                        mm_group(pg, pg[:, :], [(w(kc, half * 128, half * 128 + 128), XeT[:, kc, :]) for kc in range(16)], wb + [XeT])
                        g_ = gtmp[r]; s_ = sgt[r]
                        if kind == "g":
                            fw.op(dve, lambda e, pg=pg, g_=g_, ft=ft: e.tensor_scalar(out=g_[:], in0=pg[:, :], scalar1=bgu[:, e_, ft:ft + 1], scalar2=7.0, op0=ALU.add, op1=ALU.min), reads=[pg, bgu], writes=[g_])
                            fw.op(act, lambda e, g_=g_, s_=s_: e.activation(out=s_[:], in_=g_[:], func=AF.Sigmoid, scale=1.702), reads=[g_], writes=[s_])
                            fw.op(pool, lambda e, g_=g_, s_=s_, ft=ft: e.tensor_tensor(out=gs[:, ft, :], in0=g_[:], in1=s_[:], op=ALU.mult), reads=[g_, s_], writes=[gs])
                        else:
                            fw.op(dve, lambda e, pg=pg, g_=g_, ft=ft: e.tensor_scalar(out=g_[:], in0=pg[:, :], scalar1=bgu[:, e_, 16 + ft:17 + ft], scalar2=7.0, op0=ALU.add, op1=ALU.min), reads=[pg, bgu], writes=[g_])
                            fw.op(dve, lambda e, g_=g_, s_=s_: e.tensor_scalar(out=s_[:], in0=g_[:], scalar1=-7.0, scalar2=1.0, op0=ALU.max, op1=ALU.add), reads=[g_], writes=[s_])
                            fw.op(pool, lambda e, s_=s_, ft=ft: e.tensor_tensor(out=actT[:, ft, :], in0=s_[:], in1=gs[:, ft, :], op=ALU.mult), reads=[s_, gs], writes=[actT])
                else:
                    yb_ = ytb[ti[0] % 2]; ti[0] += 1
                    for tt in range(4):
                        py = pbank()
                        mm_group(py, py[:, 0:256], [(actT[:, kc, tt * 128:(tt + 1) * 128], w(kc, 0, 256)) for kc in range(16)], wb + [actT])
                        if tt % 2 == 0:
                            fw.op(act, lambda e, py=py, tt=tt, yb_=yb_: e.copy(out=yb_[:, tt, :], in_=py[:, 0:256]), reads=[py], writes=[yb_])
                        else:
                            fw.op(dve, lambda e, py=py, tt=tt, yb_=yb_: e.tensor_copy(out=yb_[:, tt, :], in_=py[:, 0:256]), reads=[py], writes=[yb_])
                    ld(ybuf[e_ * CAP:(e_ + 1) * CAP, j * 256:(j + 1) * 256].rearrange("(t p) d -> p t d", p=128), yb_[:], [yb_], [db("ybuf", e_, j)])

            blocks = []
            for e_ in range(NE):
                for j in range(8):
                    blocks.append((wgu[e_, j, :, :, :], 16, (e_, "g", j)))
                for j in range(8):
                    blocks.append((wgu[e_, 8 + j, :, :, :], 16, (e_, "u", j)))
                for j in range(8):
                    blocks.append((wdn[e_, j, :, :, :], 16, (e_, "d", j)))
            stream(ws, blocks, body, depth=2)
            phase_end()

        if STOP_AFTER >= 6:
          with ExitStack() as ph:
            bdn = sbt(ph, "bdn", [32, 2048], F32)
            ld(bdn[:], bdn_d[:, :], [], [bdn])
            yk = [sbt(ph, "yk", [128, 2048], F32) for _ in range(4)]
            f_ = sbt(ph, "f_", [128, 2048], F32); r2 = sbt(ph, "r2", [128, 16, 512], F32)
            au1 = sbt(ph, "au1", [128, 16, 512], F32)
            lt = ln_tiles(ph)
            allyb = [db("ybuf", e_, j_) for e_ in range(NE) for j_ in range(8)]
            for gi, (t0, n) in enumerate(OWN_GROUPS):
                ld(au1[:, :, 0:n], u1T_s[:, :, t0:t0 + n], [db("u1T", gi)], [au1])
                for tt in range(n // 128):
                    T = (t0 + tt * 128) // 128
                    c0 = tt * 128
                    for k in range(4):
                        fw.dma(pool, lambda e, T=T, k=k: e.indirect_dma_start(out=yk[k][:], out_offset=None, in_=ybuf[:, :], in_offset=bass.IndirectOffsetOnAxis(ap=rows_i[:, T, k:k + 1], axis=0)),
                               reads=allyb + [rows_i], writes=[yk[k]])
                    for q4 in range(4):
                        pb = pbank()
                        mm_group(pb, pb[:, :], [(combT[0:32, T * 128:(T + 1) * 128], bdn[0:32, q4 * 512:(q4 + 1) * 512])], [combT, bdn])
                        fw.op(dve, lambda e, pb=pb, q4=q4, T=T: e.scalar_tensor_tensor(out=f_[:, q4 * 512:(q4 + 1) * 512], in0=yk[0][:, q4 * 512:(q4 + 1) * 512], scalar=gates[:, T, 0:1], in1=pb[:, :], op0=ALU.mult, op1=ALU.add),
                              reads=[pb, yk[0], gates], writes=[f_])
                    for k in range(1, 4):
                        eng = dve
                        fw.op(eng, lambda e, k=k, T=T: e.scalar_tensor_tensor(out=f_[:], in0=yk[k][:], scalar=gates[:, T, k:k + 1], in1=f_[:], op0=ALU.mult, op1=ALU.add), reads=[yk[k], gates], writes=[f_])
                    for q4 in range(4):
                        pt = pbank()
                        tr_group(pt, [(pt[:, j * 128:(j + 1) * 128], f_[:, (q4 * 4 + j) * 128:(q4 * 4 + j + 1) * 128], ident[:]) for j in range(4)], [f_, ident])
                        for j in range(4):
                            kc = q4 * 4 + j
                            for (o, l, s) in segs_of(t0, n):
                                lo_ = max(o, c0); hi_ = min(o + l, c0 + 128)
                                if lo_ >= hi_:
                                    continue
                                fw.op(dve, lambda e, pt=pt, j=j, kc=kc, lo_=lo_, hi_=hi_, s=s, c0=c0: e.scalar_tensor_tensor(out=r2[:, kc, lo_:hi_], in0=pt[:, j * 128 + lo_ - c0:j * 128 + hi_ - c0], scalar=modT[:, 5, kc, s:s + 1], in1=au1[:, kc, lo_:hi_], op0=ALU.mult, op1=ALU.add),
                                      reads=[pt, modT, au1], writes=[r2])
                ln_core(lt, r2, n)
                affine(r2, lambda kc, o, l: r2[:, kc, o:o + l], r2, lambda kc, s: lnp[:, 4, kc:kc + 1], lambda kc, s: lnp[:, 5, kc:kc + 1], t0, n, [lnp])
                ld(yT[:, :, t0:t0 + n], r2[:, :, 0:n], [r2], [db("yT", gi)])
            phase_end()

        fw.finish()
        fw.emit_all()
    return nc


def _prep_shared(inp):
    f = lambda a: np.ascontiguousarray(a, dtype=np.float32)
    sh = {}
    sh["lnp"] = f(np.stack([inp["ln0_g"], inp["ln0_b"], inp["ln1_g"][0], inp["ln1_b"][0], inp["ln2_g"][0], inp["ln2_b"][0]]).reshape(6, 16, 128).transpose(2, 0, 1))
    sh["bada"] = f(np.asarray(inp["b_ada"])[0].reshape(6, 16, 128).transpose(2, 0, 1))
    sh["wada"] = f(np.asarray(inp["w_ada"])[0].reshape(16, 128, 24, 512).transpose(2, 1, 0, 3))
    sh["win"] = f(np.asarray(inp["w_in"])[0].reshape(16, 128, 80, 128).transpose(2, 1, 0, 3))
    sh["convw"] = f(np.asarray(inp["conv_w"])[0].reshape(3, 8, 128).transpose(2, 1, 0))
    sh["wbc"] = f(np.asarray(inp["w_br_conv"])[0].reshape(8, 128, 8, 256).transpose(2, 1, 0, 3))
    sh["wba"] = f(np.asarray(inp["w_br_att"])[0].reshape(8, 128, 8, 256).transpose(2, 1, 0, 3))
    sh["wout"] = f(np.asarray(inp["w_out"])[0].reshape(16, 128, 8, 256).transpose(2, 1, 0, 3))
    sh["wr"] = f(np.asarray(inp["w_router"])[0].reshape(16, 128, 32).transpose(1, 0, 2))
    sh["br"] = f(np.tile(np.asarray(inp["b_router"])[0][None, :], (128, 1)))
    sh["wgu"] = f(np.asarray(inp["w_gu"])[0].reshape(32, 16, 128, 16, 256).transpose(0, 3, 2, 1, 4))
    sh["bgu"] = f(np.asarray(inp["b_gu"])[0].reshape(32, 32, 128).transpose(2, 0, 1))
    sh["wdn"] = f(np.asarray(inp["w_dn"])[0].reshape(32, 16, 128, 8, 256).transpose(0, 3, 2, 1, 4))
    sh["bdn"] = f(np.asarray(inp["b_dn"])[0])
    return sh


def _fm(X):
    T = X.shape[0]
    return np.ascontiguousarray(X.T.reshape(16, 128, T).transpose(1, 0, 2), dtype=np.float32)


_NC_CACHE = {}


def kernel(**inp):
    xpr = np.asarray(inp["x_prompt"], dtype=np.float32)
    xsm = np.asarray(inp["x_sample"], dtype=np.float32)
    cpr = np.asarray(inp["c_prompt"], dtype=np.float32)
    csm = np.asarray(inp["c_sample"], dtype=np.float32)
    ckk = np.asarray(inp["cache_k"], dtype=np.float32)[0]
    cvv = np.asarray(inp["cache_v"], dtype=np.float32)[0]
    ccv = np.asarray(inp["cache_conv"], dtype=np.float32)[0]
    sh = _prep_shared(inp)
    in_maps = []
    for c in range(8):
        b, h = c // 2, c % 2
        X = np.concatenate([xpr[b, h * 2048:(h + 1) * 2048], xsm[4 * c:4 * c + 4].reshape(256, 2048)], axis=0)
        m = dict(sh)
        m["xo"] = _fm(X)
        m["xp"] = _fm(xpr[b, 0:2048]) if h == 1 else np.zeros((128, 16, 2048), np.float32)
        m["flag"] = np.full((128, 1), float(h), np.float32)
        C = np.zeros((8, 2048), np.float32)
        C[0] = cpr[b]; C[1:5] = csm[4 * c:4 * c + 4]
        m["cT"] = np.ascontiguousarray(C.T.reshape(16, 128, 8).transpose(1, 0, 2))
        m["ck"] = np.ascontiguousarray(ckk[4 * c:4 * c + 4].transpose(0, 2, 3, 1))
        m["cv"] = np.ascontiguousarray(cvv[4 * c:4 * c + 4].reshape(4, 16, 128, 8, 128).transpose(0, 3, 2, 1, 4))
        m["cconv"] = np.ascontiguousarray(ccv[4 * c:4 * c + 4].reshape(4, 2, 8, 128).transpose(3, 2, 0, 1))
        in_maps.append(m)
    if "nc" not in _NC_CACHE:
        _NC_CACHE["nc"] = build_nc()
    nc = _NC_CACHE["nc"]
    res = run_bass_kernel_spmd(nc, in_maps, core_ids=list(range(8)))
    y_prompt = np.zeros((4, 4096, 2048), np.float32); y_sample = np.zeros((32, 64, 2048), np.float32)
    k_prompt = np.zeros((1, 4, 4096, 8, 128), np.float32); v_prompt = np.zeros((1, 4, 4096, 8, 128), np.float32)
    conv_prompt = np.zeros((1, 4, 2, 1024), np.float32)
    k_sample = np.zeros((1, 32, 64, 8, 128), np.float32); v_sample = np.zeros((1, 32, 64, 8, 128), np.float32)
    conv_sample = np.zeros((1, 32, 2, 1024), np.float32)
    for c in range(8):
        b, h = c // 2, c % 2
        r = res.results[c]
        Y = r["yT"].transpose(2, 1, 0).reshape(2304, 2048)
        y_prompt[b, h * 2048:(h + 1) * 2048] = Y[:2048]
        y_sample[4 * c:4 * c + 4] = Y[2048:].reshape(4, 64, 2048)
        K = r["kTo"].transpose(2, 1, 0)
        k_prompt[0, b, h * 2048:(h + 1) * 2048] = K[:2048]
        k_sample[0, 4 * c:4 * c + 4] = K[2048:].reshape(4, 64, 8, 128)
        V = r["vo"].reshape(2304, 8, 128)
        v_prompt[0, b, h * 2048:(h + 1) * 2048] = V[:2048]
        v_sample[0, 4 * c:4 * c + 4] = V[2048:].reshape(4, 64, 8, 128)
        cvo = r["convo"].transpose(2, 3, 1, 0).reshape(5, 2, 1024)
        if h == 1:
            conv_prompt[0, b] = cvo[0]
        conv_sample[0, 4 * c:4 * c + 4] = cvo[1:5]
    return (y_prompt, y_sample, k_prompt, v_prompt, conv_prompt, k_sample, v_sample, conv_sample)
```
